# Optimizing a Trainium2 kernel written in Bass

```python
import math
import jax
import jax.numpy as jnp
from jax import lax
import numpy as np

D_MODEL = 1024
BATCH = 8
SEQ = 4096
DEPTH = 4

GRID_W = 64
CTX_LEN = 256
HEAD_DIM = 64
GROUP_HEADS = D_MODEL // (4 * HEAD_DIM)
DIFF_HEADS = GROUP_HEADS
DIFF_DV = HEAD_DIM
DIFF_DQK = HEAD_DIM // 2
GQA_HEADS = GROUP_HEADS
GQA_KV = GROUP_HEADS // 2
NA_HEADS = GROUP_HEADS
NA_WIN_ROWS = 8
NA_WIN_COLS = 16
SWA_HEADS = GROUP_HEADS
SWA_KV = GROUP_HEADS // 2
WINDOW = 128
QBLK = 128
ROPE_THETA = 10000.0
N_EXPERTS = 32
TOP_K = 4
D_FF = D_MODEL
SWIGLU_ALPHA = 1.702
SWIGLU_LIMIT = 7.0
MOE_BLK = 256
NORM_EPS = 1e-6
IN_LAYOUT = (
    (2 * DIFF_HEADS, DIFF_DQK), (2 * DIFF_HEADS, DIFF_DQK), (DIFF_HEADS, DIFF_DV),
    (GQA_HEADS, HEAD_DIM), (GQA_KV, HEAD_DIM), (GQA_KV, HEAD_DIM),
    (NA_HEADS, HEAD_DIM), (NA_HEADS, HEAD_DIM), (NA_HEADS, HEAD_DIM),
    (SWA_HEADS, HEAD_DIM), (SWA_KV, HEAD_DIM), (SWA_KV, HEAD_DIM),
)
IN_WIDTH = sum(n * d for n, d in IN_LAYOUT)
MIX_WIDTH = DIFF_HEADS * DIFF_DV + (GQA_HEADS + NA_HEADS + SWA_HEADS) * HEAD_DIM

kernel_name = 'hybrid_parallel_group_diffusion_block'


def rms_norm(x, g):
    xf = x.astype(jnp.float32)
    y = xf * lax.rsqrt(jnp.mean(xf * xf, axis=-1, keepdims=True) + NORM_EPS)
    return (y * g.astype(jnp.float32)).astype(x.dtype)


def rope_angles(n_tok, dim):
    quarter = dim // 4
    t = jnp.arange(n_tok, dtype=jnp.int32)
    inv = ROPE_THETA ** (-jnp.arange(quarter, dtype=jnp.float32) / quarter)
    ang_r = (t // GRID_W).astype(jnp.float32)[:, None] * inv
    ang_c = (t % GRID_W).astype(jnp.float32)[:, None] * inv
    return ang_r, ang_c


def _rotate(x, ang):
    x1, x2 = jnp.split(x, 2, axis=-1)
    cos = jnp.cos(ang).astype(x.dtype)
    sin = jnp.sin(ang).astype(x.dtype)
    return jnp.concatenate([x1 * cos - x2 * sin, x2 * cos + x1 * sin], axis=-1)


def apply_axial_rope(x, ang_r, ang_c):
    half = x.shape[-1] // 2
    return jnp.concatenate([_rotate(x[..., :half], ang_r), _rotate(x[..., half:], ang_c)], axis=-1)


def project_heads(h, w_in):
    p = h @ w_in
    heads, off = [], 0
    for n, d in IN_LAYOUT:
        heads.append(p[..., off:off + n * d].reshape(p.shape[0], p.shape[1], n, d).swapaxes(1, 2))
        off += n * d
    return heads


def block_softmax_attn(q, k, v, scale, sink=None):
    B, Hq, Sq, dk = q.shape
    Hkv, dv = k.shape[1], v.shape[-1]
    G = Hq // Hkv
    blk = min(QBLK, Sq)
    nb = Sq // blk
    qb = jnp.moveaxis(q.reshape(B, Hkv, G, nb, blk, dk), 3, 0)

    def one(qi):
        s = jnp.einsum('bhgqd,bhkd->bhgqk', qi, k).astype(jnp.float32) * scale
        if sink is not None:
            sk = jnp.broadcast_to(sink.astype(jnp.float32).reshape(1, Hkv, G, 1, 1), s.shape[:-1] + (1,))
            p = jax.nn.softmax(jnp.concatenate([s, sk], axis=-1), axis=-1)[..., :-1]
        else:
            p = jax.nn.softmax(s, axis=-1)
        return jnp.einsum('bhgqk,bhkd->bhgqd', p.astype(v.dtype), v)

    o = lax.map(one, qb)
    return jnp.moveaxis(o, 0, 3).reshape(B, Hq, Sq, dv)


def diff_attn(q1, q2, k1, k2, v, lam, scale):
    B, H, Sq, dk = q1.shape
    dv = v.shape[-1]
    blk = min(QBLK, Sq)
    nb = Sq // blk
    qs = jnp.moveaxis(jnp.stack([q1, q2], axis=0).reshape(2, B, H, nb, blk, dk), 3, 0)

    def one(qb):
        s1 = jnp.einsum('bhqd,bhkd->bhqk', qb[0], k1).astype(jnp.float32) * scale
        s2 = jnp.einsum('bhqd,bhkd->bhqk', qb[1], k2).astype(jnp.float32) * scale
        p = jax.nn.softmax(s1, axis=-1) - lam * jax.nn.softmax(s2, axis=-1)
        return jnp.einsum('bhqk,bhkd->bhqd', p.astype(v.dtype), v)

    o = lax.map(one, qs)
    return jnp.moveaxis(o, 0, 2).reshape(B, H, Sq, dv)


def neighborhood_attn(q, k, v, kc, vc, rpb, scale):
    B, H, S, d = q.shape
    rows = S // GRID_W
    wr = min(NA_WIN_ROWS, rows)
    wc = NA_WIN_COLS
    n_nb = wr * wc
    qg = q.reshape(B, H, rows, GRID_W, d)
    kg = k.reshape(B, H, rows, GRID_W, d)
    vg = v.reshape(B, H, rows, GRID_W, d)
    col = jnp.arange(GRID_W)
    c0 = jnp.clip(col - wc // 2, 0, GRID_W - wc)
    col_idx = c0[:, None] + jnp.arange(wc)[None, :]
    col_bias_idx = col_idx - col[:, None] + NA_WIN_COLS - 1

    def one_row(r):
        r0 = jnp.clip(r - wr // 2, 0, rows - wr)
        kn = jnp.take(lax.dynamic_slice_in_dim(kg, r0, wr, axis=2), col_idx, axis=3)
        vn = jnp.take(lax.dynamic_slice_in_dim(vg, r0, wr, axis=2), col_idx, axis=3)
        qr = lax.dynamic_index_in_dim(qg, r, axis=2, keepdims=False)
        s_nb = jnp.einsum('bhqd,bhrqcd->bhqrc', qr, kn).astype(jnp.float32) * scale
        row_bias_idx = r0 + jnp.arange(wr) - r + NA_WIN_ROWS - 1
        bias = rpb[:, row_bias_idx[None, :, None], col_bias_idx[:, None, :]]
        s_nb = (s_nb + bias.astype(jnp.float32)).reshape(B, H, GRID_W, n_nb)
        s_cx = jnp.einsum('bhqd,bhkd->bhqk', qr, kc).astype(jnp.float32) * scale
        p = jax.nn.softmax(jnp.concatenate([s_nb, s_cx], axis=-1), axis=-1).astype(v.dtype)
        vn = vn.transpose(0, 1, 3, 2, 4, 5).reshape(B, H, GRID_W, n_nb, d)
        return (jnp.einsum('bhqn,bhqnd->bhqd', p[..., :n_nb], vn)
                + jnp.einsum('bhqk,bhkd->bhqd', p[..., n_nb:], vc))

    o = lax.map(one_row, jnp.arange(rows))
    return jnp.moveaxis(o, 0, 2).reshape(B, H, S, d)


def window_attn(q, k, v, kc, vc, sink, scale):
    B, Hq, S, d = q.shape
    Hkv = k.shape[1]
    G = Hq // Hkv
    C = kc.shape[2]
    nb = S // WINDOW
    pad = ((0, 0), (0, 0), (WINDOW, WINDOW), (0, 0))
    kb = jnp.pad(k, pad).reshape(B, Hkv, nb + 2, WINDOW, d)
    vb = jnp.pad(v, pad).reshape(B, Hkv, nb + 2, WINDOW, d)
    kwin = jnp.concatenate([kb[:, :, 0:nb], kb[:, :, 1:nb + 1], kb[:, :, 2:nb + 2]], axis=3)
    vwin = jnp.concatenate([vb[:, :, 0:nb], vb[:, :, 1:nb + 1], vb[:, :, 2:nb + 2]], axis=3)
    qb = q.reshape(B, Hkv, G, nb, WINDOW, d)
    s_loc = jnp.einsum('bhgnqd,bhnkd->bhgnqk', qb, kwin).astype(jnp.float32) * scale
    qi = jnp.arange(WINDOW)[:, None]
    kj = jnp.arange(3 * WINDOW)[None, :]
    kpos = jnp.arange(nb)[:, None, None] * WINDOW - WINDOW + kj
    valid = (jnp.abs(kj - WINDOW - qi) <= WINDOW)[None] & (kpos >= 0) & (kpos < S)
    s_loc = jnp.where(valid, s_loc, -jnp.inf)
    s_ctx = jnp.einsum('bhgnqd,bhkd->bhgnqk', qb, kc).astype(jnp.float32) * scale
    s_sink = jnp.broadcast_to(sink.astype(jnp.float32).reshape(1, Hkv, G, 1, 1, 1), s_ctx.shape[:-1] + (1,))
    p = jax.nn.softmax(jnp.concatenate([s_loc, s_ctx, s_sink], axis=-1), axis=-1).astype(v.dtype)
    n_loc = 3 * WINDOW
    o = (jnp.einsum('bhgnqk,bhnkd->bhgnqd', p[..., :n_loc], vwin)
         + jnp.einsum('bhgnqk,bhkd->bhgnqd', p[..., n_loc:n_loc + C], vc))
    return o.reshape(B, Hq, S, d)


def merge_heads(outs, w_out):
    cat = jnp.concatenate([o.swapaxes(1, 2).reshape(o.shape[0], o.shape[2], -1) for o in outs], axis=-1)
    return cat @ w_out


def token_mixing(h, hc, w_in, w_out, diff_lam, diff_g, qk_g, rpb, sink, lam_init, rope, need_ctx):
    r32, c32, r64, c64 = rope
    hd_scale = HEAD_DIM ** -0.5
    (dq, dk, dv, gq, gk, gv, nq, nk, nv, wq, wk, wv) = project_heads(h, w_in)
    (dqc, dkc, dvc, gqc, gkc, gvc, nqc, nkc, nvc, wqc, wkc, wvc) = project_heads(hc, w_in)

    lf = diff_lam.astype(jnp.float32)
    lam = jnp.exp(jnp.sum(lf[0] * lf[1])) - jnp.exp(jnp.sum(lf[2] * lf[3])) + lam_init

    def pair(t):
        return t.reshape(t.shape[0], DIFF_HEADS, 2, t.shape[2], DIFF_DQK)

    def diff_out(q, k, v):
        o = diff_attn(q[:, :, 0], q[:, :, 1], k[:, :, 0], k[:, :, 1], v, lam, DIFF_DQK ** -0.5)
        return rms_norm(o, diff_g) * (1.0 - lam_init)

    dqc, dkc = pair(dqc), pair(dkc)
    dq = apply_axial_rope(pair(dq), r32, c32)
    dk = apply_axial_rope(pair(dk), r32, c32)
    o_a = diff_out(dq, jnp.concatenate([dkc, dk], axis=3), jnp.concatenate([dvc, dv], axis=2))

    gqc, gkc = rms_norm(gqc, qk_g[0]), rms_norm(gkc, qk_g[1])
    gq = apply_axial_rope(rms_norm(gq, qk_g[0]), r64, c64)
    gk = apply_axial_rope(rms_norm(gk, qk_g[1]), r64, c64)
    o_b = block_softmax_attn(gq, jnp.concatenate([gkc, gk], axis=2), jnp.concatenate([gvc, gv], axis=2), hd_scale)

    o_c = neighborhood_attn(nq, nk, nv, nkc, nvc, rpb, hd_scale)

    o_d = window_attn(apply_axial_rope(wq, r64, c64), apply_axial_rope(wk, r64, c64), wv, wkc, wvc, sink, hd_scale)

    o = merge_heads([o_a, o_b, o_c, o_d], w_out)
    oc = None
    if need_ctx:
        oc = merge_heads([
            diff_out(dqc, dkc, dvc),
            block_softmax_attn(gqc, gkc, gvc, hd_scale),
            block_softmax_attn(nqc, nkc, nvc, hd_scale),
            block_softmax_attn(wqc, wkc, wvc, hd_scale, sink),
        ], w_out)
    return o, oc


def moe_ffn(h, router_w, router_b, w_gu, b_gu, w_dn, b_dn):
    N, D = h.shape
    logits = (h @ router_w).astype(jnp.float32) + router_b.astype(jnp.float32)
    top_v, top_i = lax.top_k(logits, TOP_K)
    gates = jax.nn.softmax(top_v, axis=-1).astype(h.dtype)
    nk = N * TOP_K
    flat_e = top_i.reshape(nk)
    order = jnp.argsort(flat_e)
    e_sorted = flat_e[order]
    tok_sorted = order // TOP_K
    gate_sorted = gates.reshape(nk)[order]
    counts = jnp.bincount(flat_e, length=N_EXPERTS)
    padded = (counts + MOE_BLK - 1) // MOE_BLK * MOE_BLK
    pad_end = jnp.cumsum(padded)
    pad_start = pad_end - padded
    start = jnp.cumsum(counts) - counts
    dest = pad_start[e_sorted] + jnp.arange(nk) - start[e_sorted]
    n_blocks = -(-nk // MOE_BLK) + N_EXPERTS
    buf = jnp.zeros((n_blocks * MOE_BLK, D), h.dtype).at[dest].set(h[tok_sorted])
    block_e = jnp.minimum(jnp.searchsorted(pad_end, jnp.arange(n_blocks) * MOE_BLK, side='right'), N_EXPERTS - 1)

    def expert_block(args):
        xb, e = args
        gu = xb @ w_gu[e] + b_gu[e]
        g, u = jnp.split(gu, 2, axis=-1)
        g = jnp.minimum(g, SWIGLU_LIMIT)
        u = jnp.clip(u, -SWIGLU_LIMIT, SWIGLU_LIMIT)
        a = g * jax.nn.sigmoid(SWIGLU_ALPHA * g) * (u + 1.0)
        return a @ w_dn[e] + b_dn[e]

    yb = lax.map(expert_block, (buf.reshape(n_blocks, MOE_BLK, D), block_e))
    y_sorted = yb.reshape(-1, D)[dest] * gate_sorted[:, None]
    return jnp.zeros((N, D), h.dtype).at[tok_sorted].add(y_sorted)


def setup_inputs(seed: int = 0) -> dict:
    key = jax.random.key(seed)
    ks = jax.random.split(key, 22)
    L, D, E, F = DEPTH, D_MODEL, N_EXPERTS, D_FF

    def nrm(k, shape, s):
        return jax.random.normal(k, shape, jnp.float32) * s

    return {
        'x': nrm(ks[0], (BATCH, SEQ, D), 1.0),
        'c': nrm(ks[1], (BATCH, D), 1.0),
        'ctx': nrm(ks[2], (BATCH, CTX_LEN, D), 1.0),
        'c_ctx': nrm(ks[3], (D,), 1.0),
        'w_ada': nrm(ks[4], (L, D, 6 * D), 0.5 * D ** -0.5),
        'b_ada': nrm(ks[5], (L, 6 * D), 0.02),
        'norm1_g': 1.0 + nrm(ks[6], (L, D), 0.02),
        'norm2_g': 1.0 + nrm(ks[7], (L, D), 0.02),
        'w_in': nrm(ks[8], (L, D, IN_WIDTH), D ** -0.5),
        'w_out': nrm(ks[9], (L, MIX_WIDTH, D), MIX_WIDTH ** -0.5),
        'diff_lam': nrm(ks[10], (L, 4, DIFF_DQK), 0.1),
        'diff_subln_g': 1.0 + nrm(ks[11], (L, DIFF_DV), 0.02),
        'gqa_qk_g': 1.0 + nrm(ks[12], (L, 2, HEAD_DIM), 0.02),
        'na_rpb': nrm(ks[13], (L, NA_HEADS, 2 * NA_WIN_ROWS - 1, 2 * NA_WIN_COLS - 1), 0.1),
        'swa_sink': nrm(ks[14], (L, SWA_HEADS), 0.5),
        'router_w': nrm(ks[15], (L, D, E), D ** -0.5),
        'router_b': nrm(ks[16], (L, E), 0.01),
        'exp_w_gu': nrm(ks[17], (L, E, D, 2 * F), D ** -0.5),
        'exp_b_gu': nrm(ks[18], (L, E, 2 * F), 0.01),
        'exp_w_down': nrm(ks[19], (L, E, F, D), F ** -0.5),
        'exp_b_down': nrm(ks[20], (L, E, D), 0.01),
        'final_g': 1.0 + nrm(ks[21], (D,), 0.02),
    }


def reference(x, c, ctx, c_ctx, w_ada, b_ada, norm1_g, norm2_g, w_in, w_out, diff_lam, diff_subln_g,
              gqa_qk_g, na_rpb, swa_sink, router_w, router_b, exp_w_gu, exp_b_gu, exp_w_down, exp_b_down,
              final_g):
    B, S, D = x.shape
    r32, c32 = rope_angles(S, DIFF_DQK)
    r64, c64 = rope_angles(S, HEAD_DIM)
    rope = (r32, c32, r64, c64)
    xc = ctx
    silu_c = jax.nn.silu(c)
    silu_cc = jax.nn.silu(c_ctx)
    for l in range(DEPTH):
        need_ctx = l < DEPTH - 1
        lam_init = 0.8 - 0.6 * math.exp(-0.3 * l)
        sh1, sc1, g1, sh2, sc2, g2 = jnp.split((silu_c @ w_ada[l] + b_ada[l])[:, None, :], 6, axis=-1)
        sh1c, sc1c, g1c, sh2c, sc2c, g2c = jnp.split((silu_cc @ w_ada[l] + b_ada[l])[None, None, :], 6, axis=-1)
        h = rms_norm(x, norm1_g[l]) * (1.0 + sc1) + sh1
        hc = rms_norm(xc, norm1_g[l]) * (1.0 + sc1c) + sh1c
        o, oc = token_mixing(h, hc, w_in[l], w_out[l], diff_lam[l], diff_subln_g[l], gqa_qk_g[l], na_rpb[l],
                             swa_sink[l], lam_init, rope, need_ctx)
        x = x + g1 * o
        h2 = rms_norm(x, norm2_g[l]) * (1.0 + sc2) + sh2
        moe_w = (router_w[l], router_b[l], exp_w_gu[l], exp_b_gu[l], exp_w_down[l], exp_b_down[l])
        if need_ctx:
            xc = xc + g1c * oc
            h2c = rms_norm(xc, norm2_g[l]) * (1.0 + sc2c) + sh2c
            y = moe_ffn(jnp.concatenate([h2.reshape(-1, D), h2c.reshape(-1, D)], axis=0), *moe_w)
            x = x + g2 * y[:B * S].reshape(B, S, D)
            xc = xc + g2c * y[B * S:].reshape(xc.shape)
        else:
            x = x + g2 * moe_ffn(h2.reshape(-1, D), *moe_w).reshape(B, S, D)
    return rms_norm(x, final_g)
```

```python
import math
from contextlib import ExitStack

import numpy as np
import ml_dtypes
import concourse.bass as bass
import concourse.mybir as mybir
from concourse.bass_utils import run_bass_kernel_spmd

F32 = mybir.dt.float32
BF16 = mybir.dt.bfloat16
ALU = mybir.AluOpType
AF = mybir.ActivationFunctionType
AX = mybir.AxisListType

D = 1024
S = 4096
C = 256
NT = S + C
L = 4
E = 32
GRID = 64
EPS = 1e-6
NEG = -30000.0
CHUNKS = [(0, 256)] + [(256 + 512 * i, 512) for i in range(8)]
NQK = 20
NSW = 16
VW = 12 * 65
VWP = VW + 64
COMPUTE = ("pe", "act", "dve", "pool")
NRING = 16
import os
SPARSE = os.environ.get("MOE_SPARSE", "1") == "1"
MB = int(os.environ.get("MOE_MB", "512"))
NBLK = (NT * 4) // MB + E
NSLOT = NBLK * MB
I32 = mybir.dt.int32
ATT_DEFER = os.environ.get("ATT_DEFER", "1") == "1"
MOE_DEFER = os.environ.get("MOE_DEFER", "0") == "1"


class Prog:
    def __init__(self, nc, stack):
        self.nc = nc
        self.ops = {e: [] for e in ("pe", "act", "dve", "pool", "sp")}
        self.cnt = {e: 0 for e in COMPUTE}
        self.esem = {e: stack.enter_context(nc.semaphore("sem_" + e)) for e in COMPUTE}
        self.ring, self.ringcnt, self.ringpos = {}, {}, {}
        for q in ("sp", "pool", "act"):
            self.ring[q] = [stack.enter_context(nc.semaphore("dq_%s_%d" % (q, i))) for i in range(NRING)]
            self.ringcnt[q] = [0] * NRING
            self.ringpos[q] = 0
        self.waited = {}
        self.state = {}

    def _deps(self, eng, reads, writes):
        deps = {}

        def add(s, v):
            if deps.get(s, (None, 0))[1] < v:
                deps[s] = (s, v)

        for r in reads:
            st = self.state.get(r)
            if st is not None and st[0] is not None:
                add(*st[0])
        for w in writes:
            st = self.state.get(w)
            if st is not None:
                if st[0] is not None:
                    add(*st[0])
                for s, v in st[1].values():
                    add(s, v)
        out = []
        for s, v in deps.values():
            if eng == "pe" and s is self.esem["pe"]:
                continue
            key = (eng, id(s))
            if self.waited.get(key, 0) >= v:
                continue
            self.waited[key] = v
            out.append((s, v))
        return out

    def _commit(self, tok, reads, writes):
        for w in writes:
            self.state[w] = [tok, {}]
        for r in reads:
            st = self.state.get(r)
            if st is None:
                st = self.state[r] = [None, {}]
            s, v = tok
            if st[1].get(id(s), (None, 0))[1] < v:
                st[1][id(s)] = (s, v)

    def op(self, eng, fn, reads=(), writes=()):
        waits = self._deps(eng, reads, writes)
        self.cnt[eng] += 1
        tok = (self.esem[eng], self.cnt[eng])
        self.ops[eng].append((waits, fn, (self.esem[eng], 1)))
        self._commit(tok, reads, writes)

    def dma(self, q, out, in_, reads=(), writes=(), **kw):
        self.dmaf(q, lambda e: e.dma_start(out=out, in_=in_, **kw), reads, writes)

    def dmaf(self, q, fn, reads=(), writes=()):
        waits = self._deps(q, reads, writes)
        j = self.ringpos[q]
        self.ringpos[q] = (j + 1) % NRING
        sem = self.ring[q][j]
        prev = self.ringcnt[q][j]
        key = (q, id(sem))
        if prev > 0 and self.waited.get(key, 0) < prev:
            self.waited[key] = prev
            waits.append((sem, prev))
        self.ringcnt[q][j] = prev + 16
        tok = (sem, prev + 16)
        self.ops[q].append((waits, fn, (sem, 16)))
        self._commit(tok, reads, writes)

    def gather(self, out, in_, idx, reads, writes, element_offset=0):
        self.dmaf("pool", lambda e: e.indirect_dma_start(out=out, out_offset=None, in_=in_,
                                                         in_offset=bass.IndirectOffsetOnAxis(ap=idx, axis=0),
                                                         element_offset=element_offset), reads, writes)

    def scatter(self, out, in_, idx, reads, writes):
        self.dmaf("pool", lambda e: e.indirect_dma_start(out=out, out_offset=bass.IndirectOffsetOnAxis(ap=idx, axis=0),
                                                         in_=in_, in_offset=None), reads, writes)

    def wait_all(self, eng, keys):
        waits = self._deps(eng, keys, ())
        self.ops[eng].append((waits, None, None))

    def emit(self):
        nc = self.nc
        ops = self.ops
        self.ops = {e: [] for e in ops}

        def run(e, lst):
            for waits, fn, inc in lst:
                for s, v in waits:
                    e.wait_ge(s, v)
                if fn is not None:
                    fn(e).then_inc(inc[0], inc[1])

        with nc.Block() as block:
            @block.tensor
            def _(e):
                run(e, ops["pe"])

            @block.scalar
            def _(e):
                run(e, ops["act"])

            @block.vector
            def _(e):
                run(e, ops["dve"])

            @block.gpsimd
            def _(e):
                run(e, ops["pool"])

            @block.sync
            def _(e):
                run(e, ops["sp"])

    def mm(self, out, lhsT, rhs, start, stop, r, w):
        self.op("pe", lambda e: e.matmul(out, lhsT, rhs, start=start, stop=stop), r, w)

    def tr(self, out, in_, ident, r, w):
        self.op("pe", lambda e: e.transpose(out, in_, ident), r, w)

    def act(self, out, in_, func, r, w, bias=None, scale=None):
        kw = {}
        if bias is not None:
            kw["bias"] = bias
        if scale is not None:
            kw["scale"] = scale
        self.op("act", lambda e: e.activation(out=out, in_=in_, func=func, **kw), r, w)

    def ts(self, eng, out, in0, s1, s2, op0, op1, r, w):
        if s2 is None:
            self.op(eng, lambda e: e.tensor_scalar(out=out, in0=in0, scalar1=s1, scalar2=None, op0=op0), r, w)
        else:
            self.op(eng, lambda e: e.tensor_scalar(out=out, in0=in0, scalar1=s1, scalar2=s2, op0=op0, op1=op1), r, w)

    def tt(self, eng, out, in0, in1, op, r, w):
        self.op(eng, lambda e: e.tensor_tensor(out=out, in0=in0, in1=in1, op=op), r, w)

    def stt(self, eng, out, in0, scalar, in1, op0, op1, r, w):
        self.op(eng, lambda e: e.scalar_tensor_tensor(out=out, in0=in0, scalar=scalar, in1=in1, op0=op0, op1=op1), r, w)

    def cp(self, eng, out, in_, r, w):
        self.op(eng, lambda e: e.tensor_copy(out, in_), r, w)

    def recip(self, out, in_, r, w):
        self.op("dve", lambda e: e.reciprocal(out=out, in_=in_), r, w)

    def memset(self, eng, out, val, w):
        self.op(eng, lambda e: e.memset(out, val), (), w)


def _swap_idx(unit):
    h = unit // 2
    q = unit // 4
    idx = np.arange(unit)
    out = idx.copy()
    for g in range(2):
        b = g * h
        out[b:b + q] = idx[b + q:b + 2 * q]
        out[b + q:b + 2 * q] = idx[b:b + q]
    return out


def _rope_tables(unit, pad_to):
    quarter = unit // 4
    t = np.arange(S)
    inv = (10000.0 ** (-np.arange(quarter, dtype=np.float32) / quarter)).astype(np.float32)
    ang_r = (t // GRID).astype(np.float32)[:, None] * inv
    ang_c = (t % GRID).astype(np.float32)[:, None] * inv
    cosu = np.zeros((pad_to, NT), np.float32)
    sinu = np.zeros((pad_to, NT), np.float32)
    for d in range(unit):
        g = d // (unit // 2)
        j = d % (unit // 2)
        ang = (ang_r if g == 0 else ang_c)[:, j % quarter]
        cosu[d, :C] = 1.0
        cosu[d, C:] = np.cos(ang)
        sgn = -1.0 if j < quarter else 1.0
        sinu[d, C:] = sgn * np.sin(ang)
    rep = 128 // pad_to
    return np.tile(cosu, (rep, 1)), np.tile(sinu, (rep, 1))


def _prep_shared(inp):
    sh = {}
    sh["ident"] = np.eye(128, dtype=np.float32)
    w_in = inp["w_in"]
    off = {"dq": 0, "dk": 256, "dv": 512, "gq": 768, "gk": 1024, "gv": 1152, "nq": 1280, "nk": 1536,
           "nv": 1792, "wq": 2048, "wk": 2304, "wv": 2432}
    cols, cols_sw, zero = [], [], []
    sw32, sw64 = _swap_idx(32), _swap_idx(64)
    for nm in ("dq", "dk"):
        for u in range(8):
            base = off[nm] + 32 * u
            cols += list(base + np.arange(32)) + [0] * 32
            cols_sw += list(base + sw32) + [0] * 32
            zero += [False] * 32 + [True] * 32
    for nm, units in (("gq", (0, 1, 2, 3)), ("gk", (0, 0, 1, 1)), ("wq", (0, 1, 2, 3)), ("wk", (0, 0, 1, 1))):
        for u in units:
            base = off[nm] + 64 * u
            cols += list(base + np.arange(64))
            cols_sw += list(base + sw64)
            zero += [False] * 64
    nsw = len(cols)
    for nm in ("nq", "nk"):
        cols += list(off[nm] + np.arange(256))
        zero += [False] * 256
    cols = np.array(cols)
    zero = np.array(zero)
    wqk = w_in[:, :, cols].copy()
    wqk[:, :, zero] = 0.0
    wsw = w_in[:, :, np.array(cols_sw)].copy()
    wsw[:, :, zero[:nsw]] = 0.0
    vcols = np.concatenate([off["dv"] + np.arange(256), off["gv"] + np.arange(128),
                            off["nv"] + np.arange(256), off["wv"] + np.arange(128)])
    def pk(w):
        l, _, n = w.shape
        return np.ascontiguousarray(w.reshape(l, 8, 128, n).transpose(0, 2, 1, 3))
    sh["wqk"] = pk(wqk)
    sh["wsw"] = pk(wsw)
    sh["wv"] = pk(w_in[:, :, vcols])
    sh["wout"] = pk(inp["w_out"])
    sh["w_ada"] = inp["w_ada"]
    sh["b_ada_col"] = np.ascontiguousarray(inp["b_ada"].reshape(L, 48, 128).transpose(0, 2, 1))
    sh["n1col"] = np.ascontiguousarray(inp["norm1_g"].reshape(L, 8, 128).transpose(0, 2, 1))
    sh["n2col"] = np.ascontiguousarray(inp["norm2_g"].reshape(L, 8, 128).transpose(0, 2, 1))
    sh["fgcol"] = np.ascontiguousarray(inp["final_g"].reshape(8, 128).T)
    sh["lamT"] = np.ascontiguousarray(inp["diff_lam"].transpose(0, 2, 1))
    sh["dgcol"] = np.ascontiguousarray(inp["diff_subln_g"].reshape(L, 64, 1))
    g = inp["gqa_qk_g"]
    qkg = np.stack([np.tile(g[:, 0], (1, 2)), np.tile(g[:, 0][:, sw64], (1, 2)),
                    np.tile(g[:, 1], (1, 2)), np.tile(g[:, 1][:, sw64], (1, 2))], axis=-1)
    sh["qkg"] = np.ascontiguousarray(qkg)
    sh["sinkb"] = np.ascontiguousarray(np.broadcast_to(inp["swa_sink"][:, None, :], (L, 128, 4)))
    sh["router_w"] = pk(inp["router_w"])
    sh["rb_rep"] = np.ascontiguousarray(np.broadcast_to(inp["router_b"][:, None, :], (L, 128, E)))
    sh["wgu"] = np.ascontiguousarray(inp["exp_w_gu"].reshape(L, E, 8, 128, 2048).transpose(0, 1, 3, 2, 4))
    sh["wdn"] = np.ascontiguousarray(inp["exp_w_down"].reshape(L, E, 8, 128, D).transpose(0, 1, 3, 2, 4))
    sh["bdn"] = inp["exp_b_down"]
    sh["triu"] = np.triu(np.ones((128, 128), np.float32), 1)
    sh["su32"] = np.triu(np.ones((E, E), np.float32), 1)
    sh["iota"] = np.arange(128, dtype=np.float32).reshape(128, 1)
    sh["bgu_col"] = np.ascontiguousarray(inp["exp_b_gu"].reshape(L, E, 16, 128).transpose(0, 1, 3, 2))
    sh["bdn_col"] = np.ascontiguousarray(inp["exp_b_down"].reshape(L, E, 8, 128).transpose(0, 1, 3, 2))
    ca, sa = _rope_tables(32, 64)
    cb, sb = _rope_tables(64, 64)
    sh["rope"] = np.ascontiguousarray(np.stack([ca, sa, cb, sb], axis=0))
    rpb = inp["na_rpb"]
    qc = np.arange(64)[None, :]
    kc = np.arange(64)[:, None]
    c0 = np.clip(qc - 8, 0, 48)
    cval = (kc >= c0) & (kc < c0 + 16)
    cidx = np.clip(kc - qc + 15, 0, 30)
    cf = np.where(cval[None, None, None], rpb[:, :, :, cidx], np.float32(NEG)).astype(np.float32)
    t4 = np.full((L, 4, 128, 16, 64), NEG, np.float32)
    t4[:, :, 0:64, 0:15, :] = cf.transpose(0, 1, 3, 2, 4)
    t4[:, :, 64:128, 0:15, :] = cf.transpose(0, 1, 3, 2, 4)
    t3 = np.full((L, 4, 128, 22, 64), NEG, np.float32)
    for n in range(22):
        for half in range(2):
            dr = 17 - n + half
            if 3 <= dr <= 10:
                t3[:, :, 64 * half:64 * half + 64, n, :] = cf[:, :, dr]
    sh["nat4"] = t4
    sh["nat3"] = t3
    p = np.arange(128)[:, None]
    j = np.arange(512)[None, :]
    dm = np.zeros((128, 6, 512), np.float32)
    for o in range(6):
        dm[:, o, :] = (np.abs((o - 1) * 128 + p - j) <= 128)
    sh["dmask"] = dm.astype(ml_dtypes.bfloat16)
    return sh


def build_program(shapes, n_layers=L, debug=False, stop=None):
    nc = bass.Bass("TRN2", target_bir_lowering=False)
    dr = {}
    for k, (shp, dt) in shapes.items():
        dr[k] = nc.dram_tensor(k, list(shp), dt, kind="ExternalInput").ap()
    out = nc.dram_tensor("out", [S, D], F32, kind="ExternalOutput").ap()
    skind = "ExternalOutput" if debug else "Internal"
    XT = nc.dram_tensor("XT", [D, NT], F32, kind=skind).ap()
    QKT = nc.dram_tensor("QKT", [NQK * 128, NT], BF16, kind=skind).ap()
    V = nc.dram_tensor("V", [NT, VWP], BF16, kind=skind).ap()
    OT = nc.dram_tensor("OT", [D, NT], BF16, kind=skind).ap()
    H2T = nc.dram_tensor("H2T", [D, NT], BF16, kind=skind).ap()
    GT = nc.dram_tensor("GT", [E, NT], F32, kind=skind).ap()
    H2R = nc.dram_tensor("H2R", [NT, D], BF16, kind=skind).ap()
    XB = nc.dram_tensor("XB", [NSLOT, D], BF16, kind=skind).ap()
    YB = nc.dram_tensor("YB", [NSLOT, D], F32, kind=skind).ap()
    DBG = nc.dram_tensor("DBG", [128, 34 * 4 + 34 * 4 + 2 * NBLK], F32, kind=skind).ap() if debug else None
    XTv = XT.rearrange("(k p) t -> p k t", p=128)
    QKTv = QKT.rearrange("(j p) t -> p j t", p=128)
    OTv = OT.rearrange("(k p) t -> p k t", p=128)
    H2Tv = H2T.rearrange("(k p) t -> p k t", p=128)
    Vv = V.rearrange("(t p) c -> p t c", p=128)

    xtk = lambda ci: [("XT", ci)] + [("XTe", ci, dj) for dj in range(8)]

    with ExitStack() as top:
        P = Prog(nc, top)
        uid = [0]

        def sbt(st, name, shape, dt):
            uid[0] += 1
            return st.enter_context(nc.sbuf_tensor("s%d_%s" % (uid[0], name), shape, dt))
        psbig = top.enter_context(nc.psum_tensor("psbig", [128, 4096], F32))
        ps = [psbig[:, i * 512:(i + 1) * 512] for i in range(8)]
        ident = sbt(top, "ident", [128, 128], F32)
        onesf = sbt(top, "onesf", [128, 128], F32)
        blk = sbt(top, "blk", [128, 128], F32)
        mc = sbt(top, "mc", [128, 2, 48], F32)
        a1 = sbt(top, "a1", [128, 2, 8], F32)
        a2 = sbt(top, "a2", [128, 2, 8], F32)
        scs = sbt(top, "scs", [128, 8, 2], F32)
        Mall = sbt(top, "Mall", [128, 34, E], F32)
        Gall = sbt(top, "Gall", [128, 34, E], F32)
        D4f = sbt(top, "D4f", [128, 34, 4], F32)
        D4i = sbt(top, "D4i", [128, 34, 4], I32)
        G4 = sbt(top, "G4", [128, 34, 4], F32)
        BIDX = sbt(top, "BIDX", [128, NBLK], I32)
        EIDX = sbt(top, "EIDX", [128, NBLK], I32)
        W8i = sbt(top, "W8i", [128, NBLK, 8], I32)
        W4i = sbt(top, "W4i", [128, NBLK, 4], I32)
        triu = sbt(top, "triu", [128, 128], F32)
        su32 = sbt(top, "su32", [E, E], F32)
        iotac = sbt(top, "iotac", [128, 1], F32)
        P.dma("sp", triu[:], dr["triu"], writes=["triu"])
        P.dma("sp", su32[:], dr["su32"], writes=["su32"])
        P.dma("sp", iotac[:], dr["iota"], writes=["iotac"])
        P.dma("sp", ident[:], dr["ident"], writes=["ident"])
        P.memset("dve", onesf[:], 1.0, ["onesf"])
        P.memset("dve", blk[:], 0.0, ["blk"])
        P.memset("dve", blk[0:64, 0:64], 1.0, ["blk"])
        P.memset("dve", blk[64:128, 64:128], 1.0, ["blk"])
        P.dma("sp", scs[:], dr["cvec"], writes=["scs"])
        P.act(scs[:], scs[:], AF.Silu, ["scs"], ["scs"])

        def norm_mod(xc, N, acol, bcol, outs, sq, rstd, psb, tag, rkeys):
            for k in range(8):
                P.act(sq[:, k, :N], xc[:, k, :N], AF.Square, rkeys(k), [(tag + "sq", k)])
            for k in range(8):
                P.mm(ps[psb][:, :N], onesf[:], sq[:, k, :N], k == 0, k == 7, ["onesf", (tag + "sq", k)], [("ps", psb)])
            P.ts("dve", rstd[:, :N], ps[psb][:, :N], 1.0 / D, EPS, ALU.mult, ALU.add, [("ps", psb)], [tag + "rstd"])
            P.act(rstd[:, :N], rstd[:, :N], AF.Sqrt, [tag + "rstd"], [tag + "rstd"])
            P.recip(rstd[:, :N], rstd[:, :N], [tag + "rstd"], [tag + "rstd"])
            for k in range(8):
                P.stt("dve", sq[:, k, :N], xc[:, k, :N], acol(k), rstd[:, :N], ALU.mult, ALU.mult,
                      rkeys(k) + [tag + "rstd", "cols"], [(tag + "sq", k)])
                for oi, (ofn, okey) in enumerate(outs):
                    if bcol is None:
                        P.act(ofn(k), sq[:, k, :N], AF.Copy, [(tag + "sq", k)], [(okey, k)])
                    else:
                        P.act(ofn(k), sq[:, k, :N], AF.Identity, [(tag + "sq", k), "cols"], [(okey, k)], bias=bcol(k))

        with ExitStack() as ph:
            xin = [sbt(ph, "xin%d" % i, [128, 4, D], F32) for i in range(2)]
            xts = [sbt(ph, "xts%d" % i, [128, 8, 512], F32) for i in range(2)]
            for ci, (t0, N) in enumerate(CHUNKS):
                b = ci % 2
                ntt = N // 128
                if ci == 0:
                    src = dr["ctx"].rearrange("(t p) d -> p t d", p=128)
                else:
                    src = dr["x"][(t0 - C):(t0 - C) + N, :].rearrange("(t p) d -> p t d", p=128)
                P.dma("sp", xin[b][:, :ntt, :], src, writes=[("xin", b)])
                for k in range(8):
                    pb = k % 4
                    for tt in range(ntt):
                        P.tr(ps[pb][:, tt * 128:(tt + 1) * 128], xin[b][:, tt, k * 128:(k + 1) * 128], ident[:],
                             [("xin", b), "ident"], [("ps", pb)])
                    if k % 2 == 0:
                        P.cp("dve", xts[b][:, k, :N], ps[pb][:, :N], [("ps", pb)], [("xts", b, k)])
                    else:
                        P.act(xts[b][:, k, :N], ps[pb][:, :N], AF.Copy, [("ps", pb)], [("xts", b, k)])
                P.dma("pool", XTv[:, :, t0:t0 + N], xts[b][:, :, :N], reads=[("xts", b, k) for k in range(8)],
                      writes=[("XT", ci)])
            P.emit()

        for l in range(n_layers):
            need_ctx = l < L - 1
            lam_init = 0.8 - 0.6 * math.exp(-0.3 * l)
            with ExitStack() as ph:
                wa = [sbt(ph, "wa%d" % i, [128, 8, 512], F32) for i in range(2)]
                bcol = sbt(ph, "bcol", [128, 48], F32)
                ncol = sbt(ph, "ncol", [128, 2, 8], F32)
                P.dma("sp", bcol[:], dr["b_ada_col"][l], writes=["bcol"])
                P.dma("sp", ncol[:, 0, :], dr["n1col"][l], writes=["ncol"])
                P.dma("sp", ncol[:, 1, :], dr["n2col"][l], writes=["ncol"])
                wav = dr["w_ada"][l].rearrange("(k p) n -> p k n", p=128)
                for j in range(12):
                    b = j % 2
                    P.dma("sp", wa[b][:], wav[:, :, j * 512:(j + 1) * 512], writes=[("wa", b)])
                    for nn in range(4):
                        jj = j * 4 + nn
                        for k in range(8):
                            P.mm(ps[0][:, jj * 2:jj * 2 + 2], wa[b][:, k, nn * 128:(nn + 1) * 128], scs[:, k, :],
                                 k == 0, k == 7, [("wa", b), "scs"], [("ps", 0)])
                psv = ps[0][:, 0:96].rearrange("p (j t) -> p j t", t=2)
                for t in range(2):
                    P.tt("dve", mc[:, t, :], psv[:, :, t], bcol[:], ALU.add, [("ps", 0), "bcol"], ["cols"])
                for t in range(2):
                    P.stt("dve", a1[:, t, :], mc[:, t, 8:16], 1.0, ncol[:, 0, :], ALU.add, ALU.mult, ["cols", "ncol"], ["cols"])
                    P.stt("dve", a2[:, t, :], mc[:, t, 32:40], 1.0, ncol[:, 1, :], ALU.add, ALU.mult, ["cols", "ncol"], ["cols"])
                P.emit()
            if stop == ("A", l):
                break

            with ExitStack() as ph:
                wqk = sbt(ph, "wqk", [128, 8, NQK * 128], BF16)
                wsw = sbt(ph, "wsw", [128, 8, NSW * 128], BF16)
                wv = sbt(ph, "wv", [128, 8, 768], BF16)
                qkg = sbt(ph, "qkg", [128, 4], F32)
                xc = [sbt(ph, "xc0", [128, 8, 512], F32)] * 2
                sq = sbt(ph, "sq", [128, 8, 512], F32)
                rstd = sbt(ph, "rstd", [128, 512], F32)
                hT = [sbt(ph, "hT%d" % i, [128, 8, 512], BF16) for i in range(2)]
                rp = [sbt(ph, "rp0", [128, 4, 512], F32)] * 2
                qk = [sbt(ph, "qk%d" % i, [128, 512], BF16) for i in range(4)]
                vs = [sbt(ph, "vs%d" % i, [128, 4, VWP], BF16) for i in range(2)]
                t1 = [sbt(ph, "t1_%d" % i, [128, 512], F32) for i in range(2)]
                t2 = [sbt(ph, "t2_%d" % i, [128, 512], F32) for i in range(2)]
                sqq = sbt(ph, "sqq", [128, 512], F32)
                rq = sbt(ph, "rq", [128, 512], F32)
                P.dma("pool", wqk[:], dr["wqk"][l], writes=["wqk"])
                P.dma("pool", wsw[:], dr["wsw"][l], writes=["wsw"])
                P.dma("pool", wv[:], dr["wv"][l], writes=["wv"])
                P.dma("sp", qkg[:], dr["qkg"][l], writes=["qkg"])
                for b in range(2):
                    vv = vs[b][:, :, 0:VW].rearrange("p t (h c) -> p t h c", c=65)
                    P.memset("pool", vs[b][:, :, VW:VWP], 0.0, [("vs", b)])
                    P.memset("pool", vv[:, :, :, 64:65], 1.0, [("vs", b)])
                ropev = dr["rope"].rearrange("f p t -> p f t")
                for ci, (t0, N) in enumerate(CHUNKS):
                    b = ci % 2
                    tty = 1 if ci == 0 else 0
                    P.dma("sp", xc[b][:, :, :N], XTv[:, :, t0:t0 + N], reads=xtk(ci), writes=[("xc", 0)])
                    P.dma("sp", rp[b][:, :, :N], ropev[:, :, t0:t0 + N], writes=[("rp", 0)])
                    norm_mod(xc[b], N, lambda k: a1[:, tty, k:k + 1], lambda k: mc[:, tty, k:k + 1],
                             [(lambda k: hT[b][:, k, :N], ("hT", b))], sq, rstd, 7, "B", lambda k: [("xc", 0)])
                    hkeys = [(("hT", b), k) for k in range(8)]
                    for j in range(NQK):
                        pa = j % 2
                        pq = ps[pa]
                        for k in range(8):
                            P.mm(pq[:, :N], wqk[:, k, j * 128:(j + 1) * 128], hT[b][:, k, :N], k == 0, k == 7,
                                 ["wqk", hkeys[k]], [("ps", pa)])
                        qi = (ci * NQK + j) % 4
                        dst = qk[qi][:, :N]
                        dkey = [("qk", qi)]

                        def qstore(qi=qi, j=j, t0=t0, N=N, ci=ci):
                            P.dma("pool", QKT[j * 128:(j + 1) * 128, t0:t0 + N], qk[qi][:, :N], reads=[("qk", qi)], writes=[("QKT", ci, j)])
                        if j >= NSW:
                            P.act(dst, pq[:, :N], AF.Copy, [("ps", pa)], dkey)
                            qstore()
                            continue
                        pw = ps[2 + pa]
                        for k in range(8):
                            P.mm(pw[:, :N], wsw[:, k, j * 128:(j + 1) * 128], hT[b][:, k, :N], k == 0, k == 7,
                                 ["wsw", hkeys[k]], [("ps", 2 + pa)])
                        a, bb = t1[pa], t2[pa]
                        if j < 8:
                            P.tt("dve", a[:, :N], pq[:, :N], rp[b][:, 0, :N], ALU.mult, [("ps", pa), ("rp", 0)], [("t1", pa)])
                            P.tt("dve", bb[:, :N], pw[:, :N], rp[b][:, 1, :N], ALU.mult, [("ps", 2 + pa), ("rp", 0)], [("t2", pa)])
                            P.tt("dve", dst, a[:, :N], bb[:, :N], ALU.add, [("t1", pa), ("t2", pa)], dkey)
                            qstore()
                        elif j < 12:
                            gc = 0 if j < 10 else 2
                            P.act(sqq[:, :N], pq[:, :N], AF.Square, [("ps", pa)], ["sqq"])
                            P.mm(ps[6][:, :N], blk[:], sqq[:, :N], True, True, ["blk", "sqq"], [("ps", 6)])
                            P.ts("dve", rq[:, :N], ps[6][:, :N], 1.0 / 64, EPS, ALU.mult, ALU.add, [("ps", 6)], ["rq"])
                            P.act(rq[:, :N], rq[:, :N], AF.Sqrt, ["rq"], ["rq"])
                            P.recip(rq[:, :N], rq[:, :N], ["rq"], ["rq"])
                            P.stt("dve", a[:, :N], pq[:, :N], qkg[:, gc:gc + 1], rp[b][:, 2, :N], ALU.mult, ALU.mult,
                                  [("ps", pa), ("rp", 0), "qkg"], [("t1", pa)])
                            P.stt("dve", bb[:, :N], pw[:, :N], qkg[:, gc + 1:gc + 2], rp[b][:, 3, :N], ALU.mult, ALU.mult,
                                  [("ps", 2 + pa), ("rp", 0), "qkg"], [("t2", pa)])
                            P.tt("dve", a[:, :N], a[:, :N], bb[:, :N], ALU.add, [("t1", pa), ("t2", pa)], [("t1", pa)])
                            P.tt("dve", dst, a[:, :N], rq[:, :N], ALU.mult, [("t1", pa), "rq"], dkey)
                            qstore()
                        else:
                            P.tt("dve", a[:, :N], pq[:, :N], rp[b][:, 2, :N], ALU.mult, [("ps", pa), ("rp", 0)], [("t1", pa)])
                            P.tt("dve", bb[:, :N], pw[:, :N], rp[b][:, 3, :N], ALU.mult, [("ps", 2 + pa), ("rp", 0)], [("t2", pa)])
                            P.tt("dve", dst, a[:, :N], bb[:, :N], ALU.add, [("t1", pa), ("t2", pa)], dkey)
                            qstore()
                    ntt = N // 128
                    for tt in range(ntt):
                        vv = vs[b][:, tt, 0:VW].rearrange("p (h c) -> p h c", c=65)
                        for g, (c0, cw, pb) in enumerate(((0, 512, 4), (512, 256, 5))):
                            for k in range(8):
                                P.mm(ps[pb][:, :cw], hT[b][:, k, tt * 128:(tt + 1) * 128], wv[:, k, c0:c0 + cw], k == 0, k == 7,
                                     ["wv", hkeys[k]], [("ps", pb)])
                            h0 = c0 // 64
                            P.act(vv[:, h0:h0 + cw // 64, 0:64], ps[pb][:, :cw].rearrange("p (h c) -> p h c", c=64), AF.Copy,
                                  [("ps", pb)], [("vs", b)])
                    P.dma("pool", Vv[:, t0 // 128:t0 // 128 + ntt, :], vs[b][:, :ntt, :], reads=[("vs", b)], writes=[("V", ci)])
                P.emit()
            if stop == ("B", l):
                break

            with ExitStack() as ph:
                vt = sbt(ph, "vt", [128, 34, VWP], BF16)
                rows = [sbt(ph, "rows%d" % i, [128, NT], BF16) for i in range(2)]
                qz = [[sbt(ph, "qz%d_%d" % (i, bq), [128, NT], BF16) for bq in range(2)] for i in range(2)]
                for i in range(2):
                    P.memset("pool", qz[i][0][64:128, :], 0.0, [("qzz", i, 0)])
                    P.memset("pool", qz[i][1][0:64, :], 0.0, [("qzz", i, 1)])
                pT2 = [sbt(ph, "pT%d" % i, [128, 2, 512], BF16) for i in range(3)]
                rl = sbt(ph, "rl", [128, 512], F32)
                osb = [sbt(ph, "osb%d" % i, [128, 512], F32) for i in range(2)]
                on = [sbt(ph, "on%d" % i, [128, 512], F32) for i in range(2)]
                obf = [sbt(ph, "obf%d" % i, [128, 512], BF16) for i in range(2)]
                dif = sbt(ph, "dif", [128, 512], F32)
                dsq = sbt(ph, "dsq", [128, 512], F32)
                drs = sbt(ph, "drs", [128, 512], F32)
                dm = sbt(ph, "dm", [128, 6, 512], BF16)
                t4r = sbt(ph, "t4r", [128, 16, 64], F32)
                t3r = sbt(ph, "t3r", [128, 22, 64], F32)
                t4 = [sbt(ph, "t4_%d" % i, [128, 16, 64], BF16) for i in range(4)]
                t3 = [sbt(ph, "t3_%d" % i, [128, 22, 64], BF16) for i in range(4)]
                lamt = sbt(ph, "lamt", [32, 4], F32)
                lamp = sbt(ph, "lamp", [32, 2], F32)
                lamc = sbt(ph, "lamc", [128, 4], F32)
                dgc = sbt(ph, "dgc", [64, 1], F32)
                esk = sbt(ph, "esk", [128, 4], F32)
                allchunks = list(range(9))
                P.dma("sp", vt[:], Vv, reads=[("V", ci) for ci in allchunks], writes=["vt"])
                P.dma("sp", dm[:], dr["dmask"], writes=["dm"])
                P.dma("sp", lamt[:], dr["lamT"][l], writes=["lamt"])
                P.tt("dve", lamp[:, 0:1], lamt[:, 0:1], lamt[:, 1:2], ALU.mult, ["lamt"], ["lamp"])
                P.tt("dve", lamp[:, 1:2], lamt[:, 2:3], lamt[:, 3:4], ALU.mult, ["lamt", "lamp"], ["lamp"])
                P.mm(ps[5][:, 0:2], onesf[0:32, :], lamp[:, :], True, True, ["onesf", "lamp"], [("ps", 5)])
                P.act(lamc[:, 0:2], ps[5][:, 0:2], AF.Exp, [("ps", 5)], ["lamc"])
                P.tt("dve", lamc[:, 2:3], lamc[:, 1:2], lamc[:, 0:1], ALU.subtract, ["lamc"], ["lamc"])
                P.ts("dve", lamc[:, 2:3], lamc[:, 2:3], -lam_init, None, ALU.add, None, ["lamc"], ["lamc"])
                P.dma("sp", dgc[:], dr["dgcol"][l], writes=["dgc"])
                P.ts("dve", dgc[:], dgc[:], 1.0 - lam_init, None, ALU.mult, None, ["dgc"], ["dgc"])
                P.dma("sp", esk[:], dr["sinkb"][l], writes=["esk"])
                P.act(esk[:], esk[:], AF.Exp, ["esk"], ["esk"])
                for h in range(4):
                    P.dma("sp", t4r[:], dr["nat4"][l, h], writes=["t4r"])
                    P.act(t4[h][:], t4r[:], AF.Exp, ["t4r"], [("t4", h)])
                    P.dma("sp", t3r[:], dr["nat3"][l, h], writes=["t3r"])
                    P.act(t3[h][:], t3r[:], AF.Exp, ["t3r"], [("t3", h)])

                state = {"pass": 0, "head": 0}

                def head_rows(qj, kj, bases=(0, 64)):
                    hi = state["head"] % 2
                    state["head"] += 1
                    for bq in bases:
                        P.dma("sp", qz[hi][bq // 64][bq:bq + 64, :], QKT[qj * 128 + bq:qj * 128 + bq + 64, :],
                              reads=[("QKT", ci, qj) for ci in allchunks], writes=[("qz", hi, bq // 64)])
                    P.dma("sp", rows[hi][:], QKTv[:, kj, :], reads=[("QKT", ci, kj) for ci in allchunks], writes=[("rows", hi)])
                    return hi, hi

                def attn(qslot, kslot, base, dqk, vh, scale, ci, ktiles, fin):
                    t0, N = CHUNKS[ci]
                    pi = state["pass"] % 2
                    state["pass"] += 1
                    po = ps[2 + pi]
                    nk = len(ktiles)
                    groups = [ktiles[i:i + 2] for i in range(0, nk, 2)]
                    ng = len(groups)

                    def qk(g):
                        pb0 = (0, 6)[g % 2]
                        for j, (kt, _) in enumerate(groups[g]):
                            P.mm(ps[pb0 + j][:, :N], rows[kslot][:, kt * 128:(kt + 1) * 128],
                                 qz[qslot][base // 64][:, t0:t0 + N], True, True,
                                 [("rows", kslot), ("qz", qslot, base // 64), ("qzz", qslot, base // 64)], [("ps", pb0 + j)])

                    qk(0)
                    cnt = 0
                    for g, grp in enumerate(groups):
                        pb0 = (0, 6)[g % 2]
                        pt2 = pT2[g % 3]
                        ptk = ("pT", g % 3)
                        w = len(grp)
                        if g + 1 < ng:
                            qk(g + 1)
                        src = psbig[:, pb0 * 512:(pb0 + 2) * 512].rearrange("p (b n) -> p b n", b=2)[:, :w, :N]
                        P.act(pt2[:, :w, :N], src, AF.Exp, [("ps", pb0 + j) for j in range(w)], [ptk], scale=scale)
                        for j, (kt, mask) in enumerate(grp):
                            pt = pt2[:, j, :]
                            if mask is not None:
                                mask(pt, ptk)
                            P.mm(po[:, :N], vt[:, kt, vh * 65:vh * 65 + 128], pt[:, :N], cnt == 0, cnt == nk - 1,
                                 ["vt", ptk], [("ps", 2 + pi)])
                            cnt += 1
                        if g == min(1, ng - 1) and state.get("pending") is not None:
                            pend = state["pending"]
                            state["pending"] = None
                            pend()
                    assert state.get("pending") is None
                    state["pending"] = lambda: fin(po, ("ps", 2 + pi), pi, t0, N)
                    if not ATT_DEFER:
                        state["pending"]()
                        state["pending"] = None

                def normalize(po, pokey, pi, N, dst, dkey, sink_col=None):
                    if sink_col is not None:
                        P.ts("dve", rl[64:65, :N], po[64:65, :N], sink_col, None, ALU.add, None, [pokey, "esk"], ["rl"])
                        P.recip(rl[64:65, :N], rl[64:65, :N], ["rl"], ["rl"])
                    else:
                        P.recip(rl[64:65, :N], po[64:65, :N], [pokey], ["rl"])
                    P.mm(ps[4][0:64, :N], onesf[64:65, 0:64], rl[64:65, :N], True, True, ["onesf", "rl"], [("ps", 4)])
                    P.act(osb[pi][0:64, :N], po[0:64, :N], AF.Copy, [pokey], [("osb", pi)])
                    P.tt("dve", dst, osb[pi][0:64, :N], ps[4][0:64, :N], ALU.mult, [("osb", pi), ("ps", 4)], dkey)

                def store(pi, row0, t0, N):
                    P.dma("pool", OT[row0:row0 + 64, t0:t0 + N], obf[pi][0:64, :N], reads=[("obf", pi)], writes=[("OT", row0, t0)])

                qchunks = allchunks if need_ctx else allchunks[1:]
                allk = [(kt, None) for kt in range(34)]
                ctxk = [(0, None), (1, None)]

                for h in range(4):
                    qs, ks = head_rows(h, 4 + h)
                    for ci in qchunks:
                        kts = ctxk if ci == 0 else allk
                        for m in range(2):
                            u = 2 * h + m

                            def fin(po, pokey, pi, t0, N, m=m, h=h):
                                normalize(po, pokey, pi, N, on[m][0:64, :N], [("on", m)])
                                if m == 1:
                                    P.stt("dve", dif[0:64, :N], on[1][0:64, :N], lamc[0:64, 2:3], on[0][0:64, :N], ALU.mult, ALU.add,
                                          [("on", 0), ("on", 1), "lamc"], ["dif"])
                                    P.act(dsq[0:64, :N], dif[0:64, :N], AF.Square, ["dif"], ["dsq"])
                                    P.mm(ps[5][0:64, :N], blk[0:64, 0:64], dsq[0:64, :N], True, True, ["blk", "dsq"], [("ps", 5)])
                                    P.ts("dve", drs[0:64, :N], ps[5][0:64, :N], 1.0 / 64, EPS, ALU.mult, ALU.add, [("ps", 5)], ["drs"])
                                    P.act(drs[0:64, :N], drs[0:64, :N], AF.Sqrt, ["drs"], ["drs"])
                                    P.recip(drs[0:64, :N], drs[0:64, :N], ["drs"], ["drs"])
                                    P.stt("dve", obf[pi][0:64, :N], dif[0:64, :N], dgc[:, 0:1], drs[0:64, :N], ALU.mult, ALU.mult,
                                          ["dif", "drs", "dgc"], [("obf", pi)])
                                    store(pi, h * 64, t0, N)

                            attn(qs, ks, 64 * m, 64, h, 32 ** -0.5, ci, kts, fin)

                for h in range(4):
                    qs, ks = head_rows(8 + h // 2, 10 + h // 2, (64 * (h % 2),))
                    for ci in qchunks:
                        kts = ctxk if ci == 0 else allk

                        def fin(po, pokey, pi, t0, N, h=h):
                            normalize(po, pokey, pi, N, obf[pi][0:64, :N], [("obf", pi)])
                            store(pi, 256 + h * 64, t0, N)

                        attn(qs, ks, 64 * (h % 2), 64, 4 + h // 2, 0.125, ci, kts, fin)

                for h in range(4):
                    qs, ks = head_rows(16 + h // 2, 18 + h // 2, (64 * (h % 2),))
                    for ci in qchunks:
                        if ci == 0:
                            kts = ctxk
                        else:
                            Rc = ci - 1
                            if Rc == 0:
                                tl = list(range(0, 6))
                            elif Rc == 7:
                                tl = list(range(26, 32))
                            else:
                                tl = list(range(4 * Rc - 2, 4 * Rc + 6))
                            kts = list(ctxk)
                            for t in tl:
                                if 1 <= Rc <= 6:
                                    n0 = 10 - 2 * (t - 4 * Rc)

                                    def mask(pt, ptk, n0=n0, h=h):
                                        P.tt("dve", pt[:, :], pt[:, :], t3[h][:, n0:n0 + 8, :].rearrange("p a b -> p (a b)"), ALU.mult,
                                             [ptk, ("t3", h)], [ptk])
                                else:
                                    def mask(pt, ptk, t=t, Rc=Rc, h=h):
                                        for jq in range(8):
                                            qr = 8 * Rc + jq
                                            r0 = min(max(qr - 4, 0), 56)
                                            for half in range(2):
                                                kr = 2 * t + half
                                                slot = kr - qr + 7 if r0 <= kr <= r0 + 7 else 15
                                                P.tt("dve", pt[64 * half:64 * half + 64, jq * 64:(jq + 1) * 64],
                                                     pt[64 * half:64 * half + 64, jq * 64:(jq + 1) * 64],
                                                     t4[h][64 * half:64 * half + 64, slot, :], ALU.mult, [ptk, ("t4", h)], [ptk])
                                kts.append((t + 2, mask))

                        def fin(po, pokey, pi, t0, N, h=h):
                            normalize(po, pokey, pi, N, obf[pi][0:64, :N], [("obf", pi)])
                            store(pi, 512 + h * 64, t0, N)

                        attn(qs, ks, 64 * (h % 2), 64, 6 + h, 0.125, ci, kts, fin)

                for h in range(4):
                    qs, ks = head_rows(12 + h // 2, 14 + h // 2, (64 * (h % 2),))
                    for ci in qchunks:
                        if ci == 0:
                            kts = ctxk
                        else:
                            Qc = ci - 1
                            kts = list(ctxk)
                            for o in range(6):
                                t = 4 * Qc - 1 + o
                                if t < 0 or t > 31:
                                    continue

                                def mask(pt, ptk, o=o):
                                    P.tt("dve", pt[:, :], pt[:, :], dm[:, o, :], ALU.mult, [ptk, "dm"], [ptk])
                                kts.append((t + 2, mask))

                        def fin(po, pokey, pi, t0, N, h=h):
                            normalize(po, pokey, pi, N, obf[pi][0:64, :N], [("obf", pi)], sink_col=esk[64:65, h:h + 1])
                            store(pi, 768 + h * 64, t0, N)

                        attn(qs, ks, 64 * (h % 2), 64, 10 + h // 2, 0.125, ci, kts, fin)
                if state["pending"] is not None:
                    state["pending"]()
                state["pending"] = None
                P.emit()
            if stop == ("C", l):
                break

            dchunks = list(range(9)) if need_ctx else list(range(1, 9))
            with ExitStack() as ph:
                wo = sbt(ph, "wo", [128, 8, D], BF16)
                rw = sbt(ph, "rw", [128, 8, E], F32)
                rb = sbt(ph, "rb", [128, E], F32)
                ot = [sbt(ph, "ot%d" % i, [128, 8, 512], BF16) for i in range(2)]
                xc = [sbt(ph, "dxc%d" % i, [128, 8, 512], F32) for i in range(2)]
                xn = [sbt(ph, "dxn%d" % i, [128, 8, 512], F32) for i in range(2)]
                sq = sbt(ph, "dsq2", [128, 8, 512], F32)
                rstd = sbt(ph, "drstd", [128, 512], F32)
                h2f = sbt(ph, "h2f", [128, 8, 512], F32)
                h2b = [sbt(ph, "h2b%d" % i, [128, 8, 512], BF16) for i in range(2)]
                lg = sbt(ph, "lg", [128, E], F32)
                m8 = sbt(ph, "m8", [128, 8], F32)
                nm = sbt(ph, "nm", [128, 1], F32)
                msk0 = sbt(ph, "msk", [128, E], F32)
                ex0 = sbt(ph, "ex", [128, E], F32)
                msk, ex = msk0[:], ex0[:]
                h2r = [sbt(ph, "h2r%d" % i, [128, D], BF16) for i in range(2)]
                ssum = sbt(ph, "ssum", [128, 1], F32)
                gts = [sbt(ph, "gts%d" % i, [32, 512], F32) for i in range(2)]
                P.dma("pool", wo[:], dr["wout"][l], writes=["wo"])
                P.dma("sp", rw[:], dr["router_w"][l], writes=["rw"])
                P.dma("sp", rb[:], dr["rb_rep"][l], writes=["rb"])
                for ci in dchunks:
                    t0, N = CHUNKS[ci]
                    b = ci % 2
                    tty = 1 if ci == 0 else 0
                    otk = [("OT", r0, t0) for r0 in range(0, D, 64)]
                    P.dma("sp", ot[b][:, :, :N], OTv[:, :, t0:t0 + N], reads=otk, writes=[("ot", b)])
                    P.dma("sp", xc[b][:, :, :N], XTv[:, :, t0:t0 + N], reads=xtk(ci), writes=[("dxc", b)])
                    for j in range(8):
                        pa = j % 2
                        for k in range(8):
                            P.mm(ps[pa][:, :N], wo[:, k, j * 128:(j + 1) * 128], ot[b][:, k, :N], k == 0, k == 7,
                                 ["wo", ("ot", b)], [("ps", pa)])
                        P.stt("dve", xn[b][:, j, :N], ps[pa][:, :N], mc[:, tty, 16 + j:17 + j], xc[b][:, j, :N], ALU.mult, ALU.add,
                              [("ps", pa), ("dxc", b), "cols"], [("dxn", b, j)])
                    P.dma("pool", XTv[:, :, t0:t0 + N], xn[b][:, :, :N], reads=[("dxn", b, j) for j in range(8)], writes=[("XT", ci)])
                    norm_mod(xn[b], N, lambda k: a2[:, tty, k:k + 1], lambda k: mc[:, tty, 24 + k:25 + k],
                             [(lambda k: h2f[:, k, :N], "h2f")] + ([] if SPARSE else [(lambda k: h2b[b][:, k, :N], ("h2b", b))]), sq, rstd, 7, "D",
                             lambda k: [("dxn", b, k)])
                    if not SPARSE:
                        P.dma("pool", H2Tv[:, :, t0:t0 + N], h2b[b][:, :, :N], reads=[(("h2b", b), k) for k in range(8)], writes=[("H2T", ci)])
                    for tt in range(N // 128):
                        gi = t0 // 128 + tt
                        if SPARSE:
                            msk = Mall[:, gi, :]
                            ex = Gall[:, gi, :]
                        pl = ps[2 + tt % 2]
                        plk = ("ps", 2 + tt % 2)
                        for k in range(8):
                            P.mm(pl[:, 0:E], h2f[:, k, tt * 128:(tt + 1) * 128], rw[:, k, :], k == 0, k == 7, [("h2f", k), "rw"], [plk])
                        P.tt("dve", lg[:], pl[:, 0:E], rb[:], ALU.add, [plk, "rb"], ["lg"])
                        P.op("dve", lambda e: e.max(out=m8[:], in_=lg[:]), ["lg"], ["m8"])
                        mk, ek = (("Mall", gi), ("Gall", gi)) if SPARSE else ("msk", "ex")
                        P.ts("dve", msk, lg[:], m8[:, 3:4], None, ALU.is_ge, None, ["lg", "m8"], [mk])
                        P.ts("dve", nm[:], m8[:, 0:1], -1.0, None, ALU.mult, None, ["m8"], ["nm"])
                        P.act(ex, lg[:], AF.Exp, ["lg", "nm"], [ek], bias=nm[:, 0:1])
                        P.tt("dve", ex, ex, msk, ALU.mult, [ek, mk], [ek])
                        P.op("dve", lambda e, ex=ex: e.reduce_sum(out=ssum[:], in_=ex, axis=AX.X), [ek], ["ssum"])
                        P.recip(ssum[:], ssum[:], ["ssum"], ["ssum"])
                        P.ts("dve", ex, ex, ssum[:, 0:1], None, ALU.mult, None, [ek, "ssum"], [ek])
                        if not SPARSE:
                            P.tr(ps[4][0:E, 0:128], ex, ident[:], [ek, "ident"], [("ps", 4)])
                            P.act(gts[b][:, tt * 128:(tt + 1) * 128], ps[4][0:E, 0:128], AF.Copy, [("ps", 4)], [("gts", b)])
                        else:
                            hb = gi % 2
                            for half in range(2):
                                pb = 5 + half
                                for kk in range(4):
                                    k = half * 4 + kk
                                    P.tr(ps[pb][:, kk * 128:(kk + 1) * 128], h2f[:, k, tt * 128:(tt + 1) * 128], ident[:],
                                         [("h2f", k), "ident"], [("ps", pb)])
                                if half == 0:
                                    P.cp("dve", h2r[hb][:, 0:512], ps[pb][:, :], [("ps", pb)], [("h2r", hb, 0)])
                                else:
                                    P.act(h2r[hb][:, 512:1024], ps[pb][:, :], AF.Copy, [("ps", pb)], [("h2r", hb, 1)])
                            P.dma("pool", H2R[gi * 128:(gi + 1) * 128, :], h2r[hb][:], reads=[("h2r", hb, 0), ("h2r", hb, 1)],
                                  writes=[("H2R", gi)])
                    if not SPARSE:
                        P.dma("pool", GT[:, t0:t0 + N], gts[b][:, :N], reads=[("gts", b)], writes=[("GT", ci)])
                P.emit()
            if stop == ("D", l):
                break

            if SPARSE:
                TL = list(range(34)) if need_ctx else list(range(2, 34))
                with ExitStack() as ph:
                    cnt = sbt(ph, "cnt", [E, 1], F32)
                    pad = sbt(ph, "pad", [E, 1], F32)
                    tmpe = sbt(ph, "tmpe", [E, 1], F32)
                    padbc = sbt(ph, "padbc", [E, 128], F32)
                    pstart = sbt(ph, "pstart", [128, E], F32)
                    pend = sbt(ph, "pend", [128, E], F32)
                    base = sbt(ph, "base", [128, E], F32)
                    bef = sbt(ph, "bef", [128, NBLK], F32)
                    bef2 = sbt(ph, "bef2", [128, NBLK], F32)
                    t32 = [sbt(ph, "t32_%d" % i, [128, E], F32) for i in range(2)]
                    dst = sbt(ph, "dst", [128, E], F32)
                    g8 = sbt(ph, "g8", [128, 8], F32)
                    hrow = [sbt(ph, "hrow%d" % i, [128, D], BF16) for i in range(2)]
                    for n_, i in enumerate(TL):
                        P.mm(ps[0][0:E, 0:1], Mall[:, i, :], onesf[:, 0:1], n_ == 0, n_ == len(TL) - 1, [("Mall", i), "onesf"], [("ps", 0)])
                    P.cp("dve", cnt[:], ps[0][0:E, 0:1], [("ps", 0)], ["cnt"])
                    P.ts("dve", pad[:], cnt[:], 0.0, float(MB), ALU.is_gt, ALU.mult, ["cnt"], ["pad"])
                    for j in range(1, NT // MB + 1):
                        P.ts("dve", tmpe[:], cnt[:], float(MB * j), float(MB), ALU.is_gt, ALU.mult, ["cnt", "tmpe"], ["tmpe"])
                        P.tt("dve", pad[:], pad[:], tmpe[:], ALU.add, ["pad", "tmpe"], ["pad"])
                    P.ts("dve", padbc[:], onesf[0:E, :], pad[:, 0:1], None, ALU.mult, None, ["onesf", "pad"], ["padbc"])
                    P.mm(ps[1][:, 0:E], padbc[:], su32[:], True, True, ["padbc", "su32"], [("ps", 1)])
                    P.mm(ps[1][:, E:2 * E], padbc[:], ident[0:E, 0:E], True, True, ["padbc", "ident"], [("ps", 1)])
                    P.cp("dve", pstart[:], ps[1][:, 0:E], [("ps", 1)], ["pstart"])
                    P.tt("dve", pend[:], pstart[:], ps[1][:, E:2 * E], ALU.add, ["pstart", ("ps", 1)], ["pend"])
                    for bk in range(NBLK):
                        tb = t32[bk % 2]
                        P.ts("dve", tb[:], pend[:], float(bk * MB), None, ALU.is_le, None, ["pend"], [("t32", bk % 2)])
                        P.op("dve", lambda e, tb=tb, bk=bk: e.reduce_sum(out=bef[:, bk:bk + 1], in_=tb[:], axis=AX.X), [("t32", bk % 2)], ["bef"])
                    P.ts("dve", bef[:], bef[:], float(E - 1), float(E * l), ALU.min, ALU.add, ["bef"], ["bef"])
                    P.cp("dve", EIDX[:], bef[:], ["bef"], ["EIDX"])
                    P.ts("dve", bef2[:], bef[:], 128.0, iotac[:, 0:1], ALU.mult, ALU.add, ["bef", "iotac"], ["bef2"])
                    P.cp("dve", BIDX[:], bef2[:], ["bef2"], ["BIDX"])
                    w8f = sbt(ph, "w8f", [128, NBLK, 8], F32)
                    for k in range(8):
                        P.ts("dve", w8f[:, :, k], bef2[:], 8.0, float(k), ALU.mult, ALU.add, ["bef2"], ["w8f"])
                    P.cp("dve", W8i[:], w8f[:], ["w8f"], ["BIDX"])
                    for k in range(4):
                        P.ts("dve", w8f[:, :, k], bef2[:], 4.0, float(k), ALU.mult, ALU.add, ["bef2", "w8f"], ["w8f"])
                    P.cp("dve", W4i[:], w8f[:, :, 0:4], ["w8f"], ["BIDX"])
                    P.cp("dve", base[:], pstart[:], ["pstart"], ["base"])
                    for i in TL:
                        P.mm(ps[2][:, 0:E], triu[:], Mall[:, i, :], True, True, ["triu", ("Mall", i)], [("ps", 2)])
                        P.mm(ps[2][:, E:2 * E], onesf[:], Mall[:, i, :], True, True, ["onesf", ("Mall", i)], [("ps", 2)])
                        P.tt("dve", dst[:], ps[2][:, 0:E], base[:], ALU.add, [("ps", 2), "base"], ["dst"])
                        P.tt("dve", base[:], base[:], ps[2][:, E:2 * E], ALU.add, [("ps", 2), "base"], ["base"])
                        P.op("dve", lambda e, i=i: e.max(out=g8[:], in_=Gall[:, i, :]), [("Gall", i)], ["g8"])
                        P.cp("dve", G4[:, i, :], g8[:, 0:4], ["g8"], [("G4", i)])
                        for k in range(4):
                            tb = t32[k % 2]
                            P.ts("dve", tb[:], Gall[:, i, :], g8[:, k:k + 1], None, ALU.is_equal, None, [("Gall", i), "g8"], [("t32", k % 2)])
                            P.tt("dve", tb[:], tb[:], dst[:], ALU.mult, [("t32", k % 2), "dst"], [("t32", k % 2)])
                            P.op("dve", lambda e, tb=tb, i=i, k=k: e.reduce_sum(out=D4f[:, i, k:k + 1], in_=tb[:], axis=AX.X),
                                 [("t32", k % 2)], [("D4f", i)])
                        P.cp("dve", D4i[:, i, :], D4f[:, i, :], [("D4f", i)], [("D4i", i)])
                        hb = i % 2
                        P.dma("sp", hrow[hb][:], H2R[i * 128:(i + 1) * 128, :], reads=[("H2R", i)], writes=[("hrow", hb)])
                        for k in range(4):
                            P.scatter(XB, hrow[hb][:], D4i[:, i, k:k + 1], [("hrow", hb), ("D4i", i)] + [("XBr", bk) for bk in range(0)],
                                      [("XB", i, k)])
                    P.emit()
                if stop == ("E1", l):
                    with ExitStack() as ph:
                        dbg = sbt(ph, "dbg", [128, 34 * 4 + 34 * 4 + 2 * NBLK], F32)
                        P.cp("dve", dbg[:, 0:136], D4f[:].rearrange("p a b -> p (a b)"), [("D4f", i) for i in TL], ["dbg"])
                        P.cp("dve", dbg[:, 136:272], G4[:].rearrange("p a b -> p (a b)"), [("G4", i) for i in TL], ["dbg"])
                        P.cp("dve", dbg[:, 272:272 + NBLK], BIDX[:], ["BIDX"], ["dbg"])
                        P.cp("dve", dbg[:, 272 + NBLK:272 + 2 * NBLK], EIDX[:], ["EIDX"], ["dbg"])
                        P.dma("sp", DBG, dbg[:], reads=["dbg"], writes=[("XB", "dbg")])
                        P.emit()
                    break
                xbkeys = [("XB", i, k) for i in TL for k in range(4)]
                with ExitStack() as ph:
                    nsub = MB // 128
                    wg = [sbt(ph, "swg%d" % i, [128, 8, 2048], BF16) for i in range(2)]
                    wd = [sbt(ph, "swd%d" % i, [128, 8, D], BF16) for i in range(2)]
                    bg = [sbt(ph, "sbg%d" % i, [128, 16], F32) for i in range(2)]
                    bdb = [sbt(ph, "sbdb%d" % i, [128, D], F32) for i in range(2)]
                    xin = [sbt(ph, "sxin%d" % i, [128, nsub, D], BF16) for i in range(2)]
                    xT = [sbt(ph, "sxT%d" % i, [128, 8, MB], BF16) for i in range(2)]
                    aT = [sbt(ph, "saT%d" % i, [128, 8, MB], BF16) for i in range(2)]
                    gs = [sbt(ph, "sgs%d" % i, [128, MB], F32) for i in range(2)]
                    sg = [sbt(ph, "ssg%d" % i, [128, MB], F32) for i in range(2)]
                    us = [sbt(ph, "sus%d" % i, [128, MB], F32) for i in range(2)]
                    ysb = [sbt(ph, "sysb%d" % i, [128, D], F32) for i in range(2)]
                    idb = sbt(ph, "sidb", [128, 128], BF16)
                    P.cp("dve", idb[:], ident[:], ["ident"], ["idb"])
                    wguv = dr["wgu"].rearrange("l e p k n -> (l e p k) n")
                    wdnv = dr["wdn"].rearrange("l e p (j a) n -> (l e p j) (a n)", a=2)
                    bguv = dr["bgu_col"].rearrange("l e p f -> (l e p) f")
                    bdnv = dr["bdn"].rearrange("l e d -> (l e) d")
                    XBv = XB.rearrange("(b s p) d -> b p s d", s=nsub, p=128)
                    for bk in range(NBLK):
                        wb = bk % 2
                        for k in range(8):
                            P.gather(wg[wb][:, k, :], wguv, W8i[:, bk, k:k + 1], ["BIDX"], [("wg", wb)] if k == 0 else [("wgx", wb, k)])
                        for j in range(4):
                            P.gather(wd[wb][:, 2 * j:2 * j + 2, :].rearrange("p a n -> p (a n)"), wdnv, W4i[:, bk, j:j + 1], ["BIDX"],
                                     [("wd", wb)] if j == 0 else [("wdx", wb, j)])
                        P.gather(bg[wb][:], bguv, BIDX[:, bk:bk + 1], ["BIDX"], [("bg", wb)])
                        P.gather(bdb[wb][:], bdnv, EIDX[:, bk:bk + 1], ["EIDX"], [("bdb", wb)])
                        P.dma("sp", xin[wb][:], XBv[bk], reads=xbkeys, writes=[("xin", wb)])
                        for s_ in range(nsub):
                            pb = 6 + s_ % 2
                            pbf = ps[pb][:].bitcast(BF16)
                            for k in range(8):
                                P.tr(pbf[:, k * 128:(k + 1) * 128], xin[wb][:, s_, k * 128:(k + 1) * 128], idb[:], [("xin", wb), "idb"], [("ps", pb)])
                            src = pbf[:, 0:1024].rearrange("p (k j) -> p k j", k=8)
                            if s_ % 2 == 0:
                                P.cp("dve", xT[wb][:, :, s_ * 128:(s_ + 1) * 128], src, [("ps", pb)], [("xT", wb, s_)])
                            else:
                                P.act(xT[wb][:, :, s_ * 128:(s_ + 1) * 128], src, AF.Copy, [("ps", pb)], [("xT", wb, s_)])
                        xk = [("xT", wb, s_) for s_ in range(nsub)]
                        wgk = [("wg", wb)] + [("wgx", wb, k) for k in range(1, 8)]
                        wdk = [("wd", wb)] + [("wdx", wb, j) for j in range(1, 4)]
                        for fi in range(8):
                            pg, pu = ps[(2 * fi) % 4], ps[(2 * fi + 1) % 4]
                            pgk, puk = ("ps", (2 * fi) % 4), ("ps", (2 * fi + 1) % 4)
                            fb = fi % 2
                            for k in range(8):
                                P.mm(pg[:, :MB], wg[wb][:, k, fi * 128:(fi + 1) * 128], xT[wb][:, k, :], k == 0, k == 7, wgk + xk, [pgk])
                            for k in range(8):
                                P.mm(pu[:, :MB], wg[wb][:, k, (8 + fi) * 128:(9 + fi) * 128], xT[wb][:, k, :], k == 0, k == 7, wgk + xk, [puk])
                            P.ts("dve", gs[fb][:], pg[:, :MB], bg[wb][:, fi:fi + 1], 7.0, ALU.add, ALU.min, [pgk, ("bg", wb)], [("gs", fb)])
                            P.act(sg[fb][:], gs[fb][:], AF.Sigmoid, [("gs", fb)], [("sg", fb)], scale=1.702)
                            P.act(us[fb][:], pu[:, :MB], AF.Identity, [puk, ("bg", wb)], [("us", fb)], bias=bg[wb][:, 8 + fi:9 + fi])
                            P.ts("dve", us[fb][:], us[fb][:], 7.0, -7.0, ALU.min, ALU.max, [("us", fb)], [("us", fb)])
                            P.tt("dve", gs[fb][:], gs[fb][:], sg[fb][:], ALU.mult, [("gs", fb), ("sg", fb)], [("gs", fb)])
                            P.stt("dve", aT[wb][:, fi, :], us[fb][:], 1.0, gs[fb][:], ALU.add, ALU.mult, [("gs", fb), ("us", fb)], [("aT", wb, fi)])
                        for s_ in range(nsub):
                            yb = (bk * nsub + s_) % 2
                            for half in range(2):
                                pd = ps[4 + half]
                                pdk = ("ps", 4 + half)
                                for f in range(8):
                                    P.mm(pd[:, :], aT[wb][:, f, s_ * 128:(s_ + 1) * 128], wd[wb][:, f, half * 512:(half + 1) * 512], f == 0, f == 7,
                                         wdk + [("aT", wb, f)], [pdk])
                                P.tt("dve", ysb[yb][:, half * 512:(half + 1) * 512], pd[:, :], bdb[wb][:, half * 512:(half + 1) * 512], ALU.add,
                                     [pdk, ("bdb", wb)], [("ysb", yb, half)])
                            r0 = bk * MB + s_ * 128
                            P.dma("sp", YB[r0:r0 + 128, :], ysb[yb][:], reads=[("ysb", yb, 0), ("ysb", yb, 1)], writes=[("YB", bk, s_)])
                    P.emit()
                if stop == ("E2", l):
                    break
                ybkeys = [("YB", bk, s_) for bk in range(NBLK) for s_ in range(MB // 128)]
                with ExitStack() as ph:
                    yk = [[sbt(ph, "yk%d_%d" % (i, k), [128, D], F32) for k in range(4)] for i in range(2)]
                    yacc = [sbt(ph, "yacc%d" % i, [128, D], F32) for i in range(2)]
                    xe3 = [sbt(ph, "xe3_%d" % i, [128, 8, 512], F32) for i in range(2)]
                    yT = sbt(ph, "yT", [128, 8, 512], F32)
                    for ci in dchunks:
                        t0, N = CHUNKS[ci]
                        cb = ci % 2
                        tty = 1 if ci == 0 else 0
                        P.dma("sp", xe3[cb][:, :, :N], XTv[:, :, t0:t0 + N], reads=xtk(ci), writes=[("xe3", cb, k) for k in range(8)])
                        for tt in range(N // 128):
                            gi = t0 // 128 + tt
                            yb = gi % 2
                            for k in range(4):
                                P.gather(yk[yb][k][:], YB, D4i[:, gi, k:k + 1], ybkeys + [("D4i", gi)], [("yk", yb, k)])
                            P.ts("dve", yacc[yb][:], yk[yb][0][:], G4[:, gi, 0:1], None, ALU.mult, None, [("yk", yb, 0), ("G4", gi)], [("yacc", yb)])
                            for k in range(1, 4):
                                P.stt("dve", yacc[yb][:], yk[yb][k][:], G4[:, gi, k:k + 1], yacc[yb][:], ALU.mult, ALU.add,
                                      [("yk", yb, k), ("G4", gi), ("yacc", yb)], [("yacc", yb)])
                            for half in range(2):
                                pb = (2 * tt + half) % 4
                                for kk in range(4):
                                    k = half * 4 + kk
                                    P.tr(ps[pb][:, kk * 128:(kk + 1) * 128], yacc[yb][:, k * 128:(k + 1) * 128], ident[:],
                                         [("yacc", yb), "ident"], [("ps", pb)])
                                dstv = yT[:, half * 4:(half + 1) * 4, tt * 128:(tt + 1) * 128]
                                srcv = ps[pb][:, 0:512].rearrange("p (a b) -> p a b", a=4)
                                if half == 0:
                                    P.cp("dve", dstv, srcv, [("ps", pb)], [("yT", tt, half)])
                                else:
                                    P.act(dstv, srcv, AF.Copy, [("ps", pb)], [("yT", tt, half)])
                        ytk = [("yT", tt, half) for tt in range(N // 128) for half in range(2)]
                        for k in range(8):
                            P.stt("dve", xe3[cb][:, k, :N], yT[:, k, :N], mc[:, tty, 40 + k:41 + k], xe3[cb][:, k, :N], ALU.mult, ALU.add,
                                  ytk + [("xe3", cb, k), "cols"], [("xe3", cb, k)] + [("yTr", k)])
                        P.dma("pool", XTv[:, :, t0:t0 + N], xe3[cb][:, :, :N], reads=[("xe3", cb, k) for k in range(8)], writes=[("XT", ci)])
                    P.emit()
                if stop == ("E", l):
                    break
                continue

            groups = [[0, 1], [2, 3], [4, 5], [6, 7], [8]] if need_ctx else [[1, 2], [3, 4], [5, 6], [7, 8]]
            with ExitStack() as ph:
                wg = [sbt(ph, "wg%d" % i, [128, 8, 2048], BF16) for i in range(2)]
                wd = [sbt(ph, "wd%d" % i, [128, 8, D], BF16) for i in range(2)]
                bg = [sbt(ph, "bg%d" % i, [128, 16], F32) for i in range(2)]
                bd = [sbt(ph, "bd%d" % i, [128, 8], F32) for i in range(2)]
                h2 = sbt(ph, "h2", [128, 8, 1024], BF16)
                acc = sbt(ph, "acc", [128, 8, 1024], F32)
                aT = [sbt(ph, "aT%d" % i, [128, 8, 512], BF16) for i in range(2)]
                gb = [sbt(ph, "gb%d" % i, [128, 512], F32) for i in range(2)]
                gs = [sbt(ph, "gs%d" % i, [128, 512], F32) for i in range(2)]
                sg = [sbt(ph, "sg%d" % i, [128, 512], F32) for i in range(2)]
                us = [sbt(ph, "us%d" % i, [128, 512], F32) for i in range(2)]
                yt = [sbt(ph, "yt%d" % i, [128, 512], F32) for i in range(2)]
                xe = [sbt(ph, "xe%d" % i, [128, 512], F32) for i in range(2)]
                it = 0
                itc = 0
                for gi, grp in enumerate([] if SPARSE else groups):
                    offs = {}
                    o = 0
                    for ci in grp:
                        t0, N = CHUNKS[ci]
                        offs[ci] = o
                        P.dma("sp", h2[:, :, o:o + N], H2Tv[:, :, t0:t0 + N], reads=[("H2T", ci)], writes=[("h2", ci)])
                        o += N
                    pend_down = None
                    for e_ in range(E):
                        wb = it % 2
                        it += 1
                        wgv = dr["wgu"][l, e_]
                        wdv = dr["wdn"][l, e_]
                        P.dma("pool", wg[wb][:], wgv, writes=[("wg", wb)])
                        P.dma("pool", wd[wb][:], wdv, writes=[("wd", wb)])
                        P.dma("sp", bg[wb][:], dr["bgu_col"][l, e_], writes=[("bg", wb)])
                        P.dma("sp", bd[wb][:], dr["bdn_col"][l, e_], writes=[("bd", wb)])
                        for ci in grp:
                            t0, N = CHUNKS[ci]
                            o = offs[ci]
                            cb = itc % 2
                            itc += 1
                            P.dma("sp", gb[cb][:, :N], GT[e_:e_ + 1, t0:t0 + N].partition_broadcast(128), reads=[("GT", ci)],
                                  writes=[("gb", cb)])
                            for fi in range(8):
                                pg, pu = ps[(2 * fi) % 4], ps[(2 * fi + 1) % 4]
                                pgk, puk = ("ps", (2 * fi) % 4), ("ps", (2 * fi + 1) % 4)
                                fb = fi % 2
                                for k in range(8):
                                    P.mm(pg[:, :N], wg[wb][:, k, fi * 128:(fi + 1) * 128], h2[:, k, o:o + N], k == 0, k == 7,
                                         [("wg", wb), ("h2", ci)], [pgk])
                                for k in range(8):
                                    P.mm(pu[:, :N], wg[wb][:, k, (8 + fi) * 128:(9 + fi) * 128], h2[:, k, o:o + N], k == 0, k == 7,
                                         [("wg", wb), ("h2", ci)], [puk])
                                P.ts("dve", gs[fb][:, :N], pg[:, :N], bg[wb][:, fi:fi + 1], 7.0, ALU.add, ALU.min, [pgk, ("bg", wb)], [("gs", fb)])
                                P.act(sg[fb][:, :N], gs[fb][:, :N], AF.Sigmoid, [("gs", fb)], [("sg", fb)], scale=1.702)
                                P.act(us[fb][:, :N], pu[:, :N], AF.Identity, [puk, ("bg", wb)], [("us", fb)], bias=bg[wb][:, 8 + fi:9 + fi])
                                P.ts("dve", us[fb][:, :N], us[fb][:, :N], 7.0, -7.0, ALU.min, ALU.max, [("us", fb)], [("us", fb)])
                                P.tt("dve", gs[fb][:, :N], gs[fb][:, :N], sg[fb][:, :N], ALU.mult, [("gs", fb), ("sg", fb)], [("gs", fb)])
                                P.stt("dve", aT[cb][:, fi, :N], us[fb][:, :N], 1.0, gs[fb][:, :N], ALU.add, ALU.mult,
                                      [("gs", fb), ("us", fb)], [("aT", cb, fi)])
                                if fi == 1 and pend_down is not None:
                                    pd_ = pend_down
                                    pend_down = None
                                    pd_()

                            def down(e_=e_, wb=wb, ci=ci, N=N, o=o, cb=cb):
                                for dj in range(8):
                                    pd = ps[4 + dj % 2]
                                    pdk = ("ps", 4 + dj % 2)
                                    yb = dj % 2
                                    for f in range(8):
                                        P.mm(pd[:, :N], wd[wb][:, f, dj * 128:(dj + 1) * 128], aT[cb][:, f, :N], f == 0, f == 7,
                                             [("wd", wb), ("aT", cb, f)], [pdk])
                                    if e_ == 0:
                                        P.stt("dve", acc[:, dj, o:o + N], pd[:, :N], bd[wb][:, dj:dj + 1], gb[cb][:, :N], ALU.add, ALU.mult,
                                              [pdk, ("bd", wb), ("gb", cb)], [("acc", ci, dj)])
                                    else:
                                        P.stt("dve", yt[yb][:, :N], pd[:, :N], bd[wb][:, dj:dj + 1], gb[cb][:, :N], ALU.add, ALU.mult,
                                              [pdk, ("bd", wb), ("gb", cb)], [("yt", yb)])
                                        P.tt("dve", acc[:, dj, o:o + N], acc[:, dj, o:o + N], yt[yb][:, :N], ALU.add,
                                             [("yt", yb), ("acc", ci, dj)], [("acc", ci, dj)])
                            pend_down = down
                            if not MOE_DEFER:
                                pend_down()
                                pend_down = None
                    if pend_down is not None:
                        pend_down()
                    for ci in grp:
                        t0, N = CHUNKS[ci]
                        o = offs[ci]
                        tty = 1 if ci == 0 else 0
                        for dj in range(8):
                            xb = dj % 2
                            P.dma("sp", xe[xb][:, :N], XT[dj * 128:(dj + 1) * 128, t0:t0 + N], reads=[("XT", ci), ("XTe", ci, dj)], writes=[("xe", xb)])
                            P.stt("dve", xe[xb][:, :N], acc[:, dj, o:o + N], mc[:, tty, 40 + dj:41 + dj], xe[xb][:, :N], ALU.mult, ALU.add,
                                  [("acc", ci, dj), ("xe", xb), "cols"], [("xe", xb)])
                            P.dma("pool", XT[dj * 128:(dj + 1) * 128, t0:t0 + N], xe[xb][:, :N], reads=[("xe", xb), ("XT", ci)], writes=[("XTe", ci, dj)])
                P.emit()
            if stop == ("E", l):
                break

        if stop is None:
            with ExitStack() as ph:
                fg = sbt(ph, "fg", [128, 8], F32)
                xc = [sbt(ph, "fxc%d" % i, [128, 8, 512], F32) for i in range(2)]
                sq = sbt(ph, "fsq", [128, 8, 512], F32)
                yf = sbt(ph, "fy", [128, 8, 512], F32)
                rstd = sbt(ph, "frstd", [128, 512], F32)
                ob = [sbt(ph, "fob%d" % i, [128, 4, D], F32) for i in range(2)]
                P.dma("sp", fg[:], dr["fgcol"], writes=["cols"])
                for ci in range(1, 9):
                    t0, N = CHUNKS[ci]
                    b = ci % 2
                    P.dma("sp", xc[b][:, :, :N], XTv[:, :, t0:t0 + N], reads=xtk(ci), writes=[("fxc", b)])
                    norm_mod(xc[b], N, lambda k: fg[:, k:k + 1], None, [(lambda k: yf[:, k, :N], "fy")], sq, rstd, 7, "F",
                             lambda k: [("fxc", b)])
                    for tt in range(4):
                        for half in range(2):
                            pb = (2 * tt + half) % 4
                            for kk in range(4):
                                k = half * 4 + kk
                                P.tr(ps[pb][:, kk * 128:(kk + 1) * 128], yf[:, k, tt * 128:(tt + 1) * 128], ident[:],
                                     [("fy", k), "ident"], [("ps", pb)])
                            if half == 0:
                                P.cp("dve", ob[b][:, tt, 0:512], ps[pb][:, :], [("ps", pb)], [("fob", b, tt, 0)])
                            else:
                                P.act(ob[b][:, tt, 512:1024], ps[pb][:, :], AF.Copy, [("ps", pb)], [("fob", b, tt, 1)])
                    P.dma("sp", out[t0 - C:t0 - C + N, :].rearrange("(t p) d -> p t d", p=128), ob[b][:],
                          reads=[("fob", b, tt, hh) for tt in range(4) for hh in range(2)], writes=[("out", ci)])
                P.wait_all("sp", [("out", ci) for ci in range(1, 9)])
                P.emit()
        else:
            keys = [k for k in P.state.keys() if isinstance(k, tuple) and k[0] in ("XT", "XTe", "QKT", "V", "OT", "H2T", "GT", "H2R", "XB", "YB")]
            P.wait_all("sp", keys)
            P.wait_all("pool", keys)
            P.emit()
    return nc


_DT = {np.dtype(np.float32): F32, np.dtype(ml_dtypes.bfloat16): BF16}


def _run(inputs, n_layers=L, debug=False, stop=None, cores=8):
    inp = {k: np.asarray(v) for k, v in inputs.items()}
    sh = _prep_shared(inp)
    in_maps = []
    for b in range(cores):
        m = dict(sh)
        m["x"] = np.ascontiguousarray(inp["x"][b])
        m["ctx"] = np.ascontiguousarray(inp["ctx"][b])
        cv = np.stack([inp["c"][b].reshape(8, 128).T, inp["c_ctx"].reshape(8, 128).T], axis=-1)
        m["cvec"] = np.ascontiguousarray(cv.astype(np.float32))
        in_maps.append(m)
    shapes = {k: (v.shape, _DT[v.dtype]) for k, v in in_maps[0].items()}
    nc = build_program(shapes, n_layers=n_layers, debug=debug, stop=stop)
    res = run_bass_kernel_spmd(nc, in_maps, core_ids=list(range(cores)))
    return res


def kernel(**inputs):
    res = _run(inputs)
    return np.stack([np.asarray(r["out"], dtype=np.float32) for r in res.results], axis=0)
```

```python
import math
from contextlib import ExitStack

import numpy as np
import ml_dtypes
import concourse.bass as bass
import concourse.mybir as mybir
from concourse.bass_utils import run_bass_kernel_spmd

F32 = mybir.dt.float32
BF16 = mybir.dt.bfloat16
ALU = mybir.AluOpType
AF = mybir.ActivationFunctionType
AX = mybir.AxisListType

D = 1024
S = 4096
C = 256
NT = S + C
L = 4
E = 32
GRID = 64
EPS = 1e-6
NEG = -30000.0
CHUNKS = [(0, 256)] + [(256 + 512 * i, 512) for i in range(8)]
NQK = 20
NSW = 16
VW = 12 * 65
VWP = VW + 64
COMPUTE = ("pe", "act", "dve", "pool")
NRING = 24
import os
SPARSE = os.environ.get("MOE_SPARSE", "1") == "1"
MB = int(os.environ.get("MOE_MB", "512"))
NBLK = (NT * 4) // MB + E
NSLOT = NBLK * MB
I32 = mybir.dt.int32
ATT_DEFER = os.environ.get("ATT_DEFER", "1") == "1"
MOE_DEFER = os.environ.get("MOE_DEFER", "0") == "1"


class Prog:
    def __init__(self, nc, stack):
        self.nc = nc
        self.ops = {e: [] for e in ("pe", "act", "dve", "pool", "sp")}
        self.cnt = {e: 0 for e in COMPUTE}
        self.esem = {e: stack.enter_context(nc.semaphore("sem_" + e)) for e in COMPUTE}
        self.ring, self.ringcnt, self.ringpos = {}, {}, {}
        for q in ("sp", "pool", "act"):
            self.ring[q] = [stack.enter_context(nc.semaphore("dq_%s_%d" % (q, i))) for i in range(NRING)]
            self.ringcnt[q] = [0] * NRING
            self.ringpos[q] = 0
        self.waited = {}
        self.state = {}

    def _deps(self, eng, reads, writes):
        deps = {}

        def add(s, v):
            if deps.get(s, (None, 0))[1] < v:
                deps[s] = (s, v)

        for r in reads:
            st = self.state.get(r)
            if st is not None and st[0] is not None:
                add(*st[0])
        for w in writes:
            st = self.state.get(w)
            if st is not None:
                if st[0] is not None:
                    add(*st[0])
                for s, v in st[1].values():
                    add(s, v)
        out = []
        for s, v in deps.values():
            if eng == "pe" and s is self.esem["pe"]:
                continue
            key = (eng, id(s))
            if self.waited.get(key, 0) >= v:
                continue
            self.waited[key] = v
            out.append((s, v))
        return out

    def _commit(self, tok, reads, writes):
        for w in writes:
            self.state[w] = [tok, {}]
        for r in reads:
            st = self.state.get(r)
            if st is None:
                st = self.state[r] = [None, {}]
            s, v = tok
            if st[1].get(id(s), (None, 0))[1] < v:
                st[1][id(s)] = (s, v)

    def op(self, eng, fn, reads=(), writes=()):
        waits = self._deps(eng, reads, writes)
        self.cnt[eng] += 1
        tok = (self.esem[eng], self.cnt[eng])
        self.ops[eng].append((waits, fn, (self.esem[eng], 1)))
        self._commit(tok, reads, writes)

    def dma(self, q, out, in_, reads=(), writes=(), **kw):
        self.dmaf(q, lambda e: e.dma_start(out=out, in_=in_, **kw), reads, writes)

    def dmaf(self, q, fn, reads=(), writes=()):
        waits = self._deps(q, reads, writes)
        j = self.ringpos[q]
        self.ringpos[q] = (j + 1) % NRING
        sem = self.ring[q][j]
        prev = self.ringcnt[q][j]
        key = (q, id(sem))
        if prev > 0 and self.waited.get(key, 0) < prev:
            self.waited[key] = prev
            waits.append((sem, prev))
        self.ringcnt[q][j] = prev + 16
        tok = (sem, prev + 16)
        self.ops[q].append((waits, fn, (sem, 16)))
        self._commit(tok, reads, writes)

    def gather(self, out, in_, idx, reads, writes, element_offset=0):
        self.dmaf("pool", lambda e: e.indirect_dma_start(out=out, out_offset=None, in_=in_,
                                                         in_offset=bass.IndirectOffsetOnAxis(ap=idx, axis=0),
                                                         element_offset=element_offset), reads, writes)

    def scatter(self, out, in_, idx, reads, writes):
        self.dmaf("pool", lambda e: e.indirect_dma_start(out=out, out_offset=bass.IndirectOffsetOnAxis(ap=idx, axis=0),
                                                         in_=in_, in_offset=None), reads, writes)

    def wait_all(self, eng, keys):
        waits = self._deps(eng, keys, ())
        self.ops[eng].append((waits, None, None))

    def emit(self):
        nc = self.nc
        ops = self.ops
        self.ops = {e: [] for e in ops}

        def run(e, lst):
            for waits, fn, inc in lst:
                for s, v in waits:
                    e.wait_ge(s, v)
                if fn is not None:
                    fn(e).then_inc(inc[0], inc[1])

        with nc.Block() as block:
            @block.tensor
            def _(e):
                run(e, ops["pe"])

            @block.scalar
            def _(e):
                run(e, ops["act"])

            @block.vector
            def _(e):
                run(e, ops["dve"])

            @block.gpsimd
            def _(e):
                run(e, ops["pool"])

            @block.sync
            def _(e):
                run(e, ops["sp"])

    def mm(self, out, lhsT, rhs, start, stop, r, w):
        self.op("pe", lambda e: e.matmul(out, lhsT, rhs, start=start, stop=stop), r, w)

    def tr(self, out, in_, ident, r, w):
        self.op("pe", lambda e: e.transpose(out, in_, ident), r, w)

    def act(self, out, in_, func, r, w, bias=None, scale=None):
        kw = {}
        if bias is not None:
            kw["bias"] = bias
        if scale is not None:
            kw["scale"] = scale
        self.op("act", lambda e: e.activation(out=out, in_=in_, func=func, **kw), r, w)

    def ts(self, eng, out, in0, s1, s2, op0, op1, r, w):
        if s2 is None:
            self.op(eng, lambda e: e.tensor_scalar(out=out, in0=in0, scalar1=s1, scalar2=None, op0=op0), r, w)
        else:
            self.op(eng, lambda e: e.tensor_scalar(out=out, in0=in0, scalar1=s1, scalar2=s2, op0=op0, op1=op1), r, w)

    def tt(self, eng, out, in0, in1, op, r, w):
        self.op(eng, lambda e: e.tensor_tensor(out=out, in0=in0, in1=in1, op=op), r, w)

    def stt(self, eng, out, in0, scalar, in1, op0, op1, r, w):
        self.op(eng, lambda e: e.scalar_tensor_tensor(out=out, in0=in0, scalar=scalar, in1=in1, op0=op0, op1=op1), r, w)

    def cp(self, eng, out, in_, r, w):
        self.op(eng, lambda e: e.tensor_copy(out, in_), r, w)

    def recip(self, out, in_, r, w):
        self.op("dve", lambda e: e.reciprocal(out=out, in_=in_), r, w)

    def memset(self, eng, out, val, w):
        self.op(eng, lambda e: e.memset(out, val), (), w)


def _swap_idx(unit):
    h = unit // 2
    q = unit // 4
    idx = np.arange(unit)
    out = idx.copy()
    for g in range(2):
        b = g * h
        out[b:b + q] = idx[b + q:b + 2 * q]
        out[b + q:b + 2 * q] = idx[b:b + q]
    return out


def _rope_tables(unit, pad_to):
    quarter = unit // 4
    t = np.arange(S)
    inv = (10000.0 ** (-np.arange(quarter, dtype=np.float32) / quarter)).astype(np.float32)
    ang_r = (t // GRID).astype(np.float32)[:, None] * inv
    ang_c = (t % GRID).astype(np.float32)[:, None] * inv
    cosu = np.zeros((pad_to, NT), np.float32)
    sinu = np.zeros((pad_to, NT), np.float32)
    for d in range(unit):
        g = d // (unit // 2)
        j = d % (unit // 2)
        ang = (ang_r if g == 0 else ang_c)[:, j % quarter]
        cosu[d, :C] = 1.0
        cosu[d, C:] = np.cos(ang)
        sgn = -1.0 if j < quarter else 1.0
        sinu[d, C:] = sgn * np.sin(ang)
    rep = 128 // pad_to
    return np.tile(cosu, (rep, 1)), np.tile(sinu, (rep, 1))


def _prep_shared(inp):
    sh = {}
    sh["ident"] = np.eye(128, dtype=np.float32)
    w_in = inp["w_in"]
    off = {"dq": 0, "dk": 256, "dv": 512, "gq": 768, "gk": 1024, "gv": 1152, "nq": 1280, "nk": 1536,
           "nv": 1792, "wq": 2048, "wk": 2304, "wv": 2432}
    cols, cols_sw, zero = [], [], []
    sw32, sw64 = _swap_idx(32), _swap_idx(64)
    for nm in ("dq", "dk"):
        for u in range(8):
            base = off[nm] + 32 * u
            cols += list(base + np.arange(32)) + [0] * 32
            cols_sw += list(base + sw32) + [0] * 32
            zero += [False] * 32 + [True] * 32
    for nm, units in (("gq", (0, 1, 2, 3)), ("gk", (0, 0, 1, 1)), ("wq", (0, 1, 2, 3)), ("wk", (0, 0, 1, 1))):
        for u in units:
            base = off[nm] + 64 * u
            cols += list(base + np.arange(64))
            cols_sw += list(base + sw64)
            zero += [False] * 64
    nsw = len(cols)
    for nm in ("nq", "nk"):
        cols += list(off[nm] + np.arange(256))
        zero += [False] * 256
    cols = np.array(cols)
    zero = np.array(zero)
    wqk = w_in[:, :, cols].copy()
    wqk[:, :, zero] = 0.0
    wsw = w_in[:, :, np.array(cols_sw)].copy()
    wsw[:, :, zero[:nsw]] = 0.0
    vcols = np.concatenate([off["dv"] + np.arange(256), off["gv"] + np.arange(128),
                            off["nv"] + np.arange(256), off["wv"] + np.arange(128)])
    def pk(w):
        l, _, n = w.shape
        return np.ascontiguousarray(w.reshape(l, 8, 128, n).transpose(0, 2, 1, 3))
    sh["wqk"] = pk(wqk)
    sh["wsw"] = pk(wsw)
    sh["wv"] = pk(w_in[:, :, vcols])
    sh["wout"] = pk(inp["w_out"])
    sh["w_ada"] = inp["w_ada"]
    sh["b_ada_col"] = np.ascontiguousarray(inp["b_ada"].reshape(L, 48, 128).transpose(0, 2, 1))
    sh["n1col"] = np.ascontiguousarray(inp["norm1_g"].reshape(L, 8, 128).transpose(0, 2, 1))
    sh["n2col"] = np.ascontiguousarray(inp["norm2_g"].reshape(L, 8, 128).transpose(0, 2, 1))
    sh["fgcol"] = np.ascontiguousarray(inp["final_g"].reshape(8, 128).T)
    sh["lamT"] = np.ascontiguousarray(inp["diff_lam"].transpose(0, 2, 1))
    sh["dgcol"] = np.ascontiguousarray(inp["diff_subln_g"].reshape(L, 64, 1))
    g = inp["gqa_qk_g"]
    qkg = np.stack([np.tile(g[:, 0], (1, 2)), np.tile(g[:, 0][:, sw64], (1, 2)),
                    np.tile(g[:, 1], (1, 2)), np.tile(g[:, 1][:, sw64], (1, 2))], axis=-1)
    sh["qkg"] = np.ascontiguousarray(qkg)
    sh["sinkb"] = np.ascontiguousarray(np.broadcast_to(inp["swa_sink"][:, None, :], (L, 128, 4)))
    sh["router_w"] = pk(inp["router_w"])
    sh["rb_rep"] = np.ascontiguousarray(np.broadcast_to(inp["router_b"][:, None, :], (L, 128, E)))
    sh["wgu"] = np.ascontiguousarray(inp["exp_w_gu"].reshape(L, E, 8, 128, 2048).transpose(0, 1, 3, 2, 4))
    sh["wdn"] = np.ascontiguousarray(inp["exp_w_down"].reshape(L, E, 8, 128, D).transpose(0, 1, 3, 2, 4))
    sh["bdn"] = inp["exp_b_down"]
    sh["triu"] = np.triu(np.ones((128, 128), np.float32), 1)
    sh["su32"] = np.triu(np.ones((E, E), np.float32), 1)
    sh["iota"] = np.arange(128, dtype=np.float32).reshape(128, 1)
    sh["bgu_col"] = np.ascontiguousarray(inp["exp_b_gu"].reshape(L, E, 16, 128).transpose(0, 1, 3, 2))
    sh["bdn_col"] = np.ascontiguousarray(inp["exp_b_down"].reshape(L, E, 8, 128).transpose(0, 1, 3, 2))
    ca, sa = _rope_tables(32, 64)
    cb, sb = _rope_tables(64, 64)
    sh["rope"] = np.ascontiguousarray(np.stack([ca, sa, cb, sb], axis=0))
    rpb = inp["na_rpb"]
    qc = np.arange(64)[None, :]
    kc = np.arange(64)[:, None]
    c0 = np.clip(qc - 8, 0, 48)
    cval = (kc >= c0) & (kc < c0 + 16)
    cidx = np.clip(kc - qc + 15, 0, 30)
    cf = np.where(cval[None, None, None], rpb[:, :, :, cidx], np.float32(NEG)).astype(np.float32)
    t4 = np.full((L, 4, 128, 16, 64), NEG, np.float32)
    t4[:, :, 0:64, 0:15, :] = cf.transpose(0, 1, 3, 2, 4)
    t4[:, :, 64:128, 0:15, :] = cf.transpose(0, 1, 3, 2, 4)
    t3 = np.full((L, 4, 128, 22, 64), NEG, np.float32)
    for n in range(22):
        for half in range(2):
            dr = 17 - n + half
            if 3 <= dr <= 10:
                t3[:, :, 64 * half:64 * half + 64, n, :] = cf[:, :, dr]
    sh["nat4"] = t4
    sh["nat3"] = t3
    p = np.arange(128)[:, None]
    j = np.arange(512)[None, :]
    dm = np.zeros((128, 6, 512), np.float32)
    for o in range(6):
        dm[:, o, :] = (np.abs((o - 1) * 128 + p - j) <= 128)
    sh["dmask"] = dm.astype(ml_dtypes.bfloat16)
    return sh


def build_program(shapes, n_layers=L, debug=False, stop=None):
    nc = bass.Bass("TRN2", target_bir_lowering=False)
    dr = {}
    for k, (shp, dt) in shapes.items():
        dr[k] = nc.dram_tensor(k, list(shp), dt, kind="ExternalInput").ap()
    out = nc.dram_tensor("out", [S, D], F32, kind="ExternalOutput").ap()
    skind = "ExternalOutput" if debug else "Internal"
    XT = nc.dram_tensor("XT", [D, NT], F32, kind=skind).ap()
    QKT = nc.dram_tensor("QKT", [NQK * 128, NT], BF16, kind=skind).ap()
    V = nc.dram_tensor("V", [NT, VWP], BF16, kind=skind).ap()
    OT = nc.dram_tensor("OT", [D, NT], BF16, kind=skind).ap()
    H2T = nc.dram_tensor("H2T", [D, NT], BF16, kind=skind).ap()
    GT = nc.dram_tensor("GT", [E, NT], F32, kind=skind).ap()
    H2R = nc.dram_tensor("H2R", [NT, D], BF16, kind=skind).ap()
    XB = nc.dram_tensor("XB", [NSLOT, D], BF16, kind=skind).ap()
    YB = nc.dram_tensor("YB", [NSLOT, D], F32, kind=skind).ap()
    DBG = nc.dram_tensor("DBG", [128, 34 * 4 + 34 * 4 + 2 * NBLK], F32, kind=skind).ap() if debug else None
    XTv = XT.rearrange("(k p) t -> p k t", p=128)
    QKTv = QKT.rearrange("(j p) t -> p j t", p=128)
    OTv = OT.rearrange("(k p) t -> p k t", p=128)
    H2Tv = H2T.rearrange("(k p) t -> p k t", p=128)
    Vv = V.rearrange("(t p) c -> p t c", p=128)

    xtk = lambda ci: [("XT", ci)] + [("XTe", ci, dj) for dj in range(8)]

    with ExitStack() as top:
        P = Prog(nc, top)
        uid = [0]

        def sbt(st, name, shape, dt):
            uid[0] += 1
            return st.enter_context(nc.sbuf_tensor("s%d_%s" % (uid[0], name), shape, dt))
        psbig = top.enter_context(nc.psum_tensor("psbig", [128, 4096], F32))
        ps = [psbig[:, i * 512:(i + 1) * 512] for i in range(8)]
        ident = sbt(top, "ident", [128, 128], F32)
        onesf = sbt(top, "onesf", [128, 128], F32)
        blk = sbt(top, "blk", [128, 128], F32)
        mc = sbt(top, "mc", [128, 2, 48], F32)
        a1 = sbt(top, "a1", [128, 2, 8], F32)
        a2 = sbt(top, "a2", [128, 2, 8], F32)
        scs = sbt(top, "scs", [128, 8, 2], F32)
        Mall = sbt(top, "Mall", [128, 34, E], F32)
        Gall = sbt(top, "Gall", [128, 34, E], F32)
        D4f = sbt(top, "D4f", [128, 34, 4], F32)
        D4i = sbt(top, "D4i", [128, 34, 4], I32)
        G4 = sbt(top, "G4", [128, 34, 4], F32)
        BIDX = sbt(top, "BIDX", [128, NBLK], I32)
        EIDX = sbt(top, "EIDX", [128, NBLK], I32)
        W8i = sbt(top, "W8i", [128, NBLK, 8], I32)
        W4i = sbt(top, "W4i", [128, NBLK, 4], I32)
        triu = sbt(top, "triu", [128, 128], F32)
        su32 = sbt(top, "su32", [E, E], F32)
        iotac = sbt(top, "iotac", [128, 1], F32)
        P.dma("sp", triu[:], dr["triu"], writes=["triu"])
        P.dma("sp", su32[:], dr["su32"], writes=["su32"])
        P.dma("sp", iotac[:], dr["iota"], writes=["iotac"])
        P.dma("sp", ident[:], dr["ident"], writes=["ident"])
        P.memset("dve", onesf[:], 1.0, ["onesf"])
        P.memset("dve", blk[:], 0.0, ["blk"])
        P.memset("dve", blk[0:64, 0:64], 1.0, ["blk"])
        P.memset("dve", blk[64:128, 64:128], 1.0, ["blk"])
        P.dma("sp", scs[:], dr["cvec"], writes=["scs"])
        P.act(scs[:], scs[:], AF.Silu, ["scs"], ["scs"])

        def norm_mod(xc, N, acol, bcol, outs, sq, rstd, psb, tag, rkeys):
            for k in range(8):
                P.act(sq[:, k, :N], xc[:, k, :N], AF.Square, rkeys(k), [(tag + "sq", k)])
            for k in range(8):
                P.mm(ps[psb][:, :N], onesf[:], sq[:, k, :N], k == 0, k == 7, ["onesf", (tag + "sq", k)], [("ps", psb)])
            P.ts("dve", rstd[:, :N], ps[psb][:, :N], 1.0 / D, EPS, ALU.mult, ALU.add, [("ps", psb)], [tag + "rstd"])
            P.act(rstd[:, :N], rstd[:, :N], AF.Sqrt, [tag + "rstd"], [tag + "rstd"])
            P.recip(rstd[:, :N], rstd[:, :N], [tag + "rstd"], [tag + "rstd"])
            for k in range(8):
                P.stt("dve", sq[:, k, :N], xc[:, k, :N], acol(k), rstd[:, :N], ALU.mult, ALU.mult,
                      rkeys(k) + [tag + "rstd", "cols"], [(tag + "sq", k)])
                for oi, (ofn, okey) in enumerate(outs):
                    if bcol is None:
                        P.act(ofn(k), sq[:, k, :N], AF.Copy, [(tag + "sq", k)], [(okey, k)])
                    else:
                        P.act(ofn(k), sq[:, k, :N], AF.Identity, [(tag + "sq", k), "cols"], [(okey, k)], bias=bcol(k))

        with ExitStack() as ph:
            xin = [sbt(ph, "xin%d" % i, [128, 4, D], F32) for i in range(2)]
            xts = [sbt(ph, "xts%d" % i, [128, 8, 512], F32) for i in range(2)]
            for ci, (t0, N) in enumerate(CHUNKS):
                b = ci % 2
                ntt = N // 128
                if ci == 0:
                    src = dr["ctx"].rearrange("(t p) d -> p t d", p=128)
                else:
                    src = dr["x"][(t0 - C):(t0 - C) + N, :].rearrange("(t p) d -> p t d", p=128)
                P.dma("sp", xin[b][:, :ntt, :], src, writes=[("xin", b)])
                for k in range(8):
                    pb = k % 4
                    for tt in range(ntt):
                        P.tr(ps[pb][:, tt * 128:(tt + 1) * 128], xin[b][:, tt, k * 128:(k + 1) * 128], ident[:],
                             [("xin", b), "ident"], [("ps", pb)])
                    if k % 2 == 0:
                        P.cp("dve", xts[b][:, k, :N], ps[pb][:, :N], [("ps", pb)], [("xts", b, k)])
                    else:
                        P.act(xts[b][:, k, :N], ps[pb][:, :N], AF.Copy, [("ps", pb)], [("xts", b, k)])
                P.dma("pool", XTv[:, :, t0:t0 + N], xts[b][:, :, :N], reads=[("xts", b, k) for k in range(8)],
                      writes=[("XT", ci)])
            P.emit()

        for l in range(n_layers):
            need_ctx = l < L - 1
            lam_init = 0.8 - 0.6 * math.exp(-0.3 * l)
            with ExitStack() as ph:
                wa = [sbt(ph, "wa%d" % i, [128, 8, 512], F32) for i in range(2)]
                bcol = sbt(ph, "bcol", [128, 48], F32)
                ncol = sbt(ph, "ncol", [128, 2, 8], F32)
                P.dma("sp", bcol[:], dr["b_ada_col"][l], writes=["bcol"])
                P.dma("sp", ncol[:, 0, :], dr["n1col"][l], writes=["ncol"])
                P.dma("sp", ncol[:, 1, :], dr["n2col"][l], writes=["ncol"])
                wav = dr["w_ada"][l].rearrange("(k p) n -> p k n", p=128)
                for j in range(12):
                    b = j % 2
                    P.dma("sp", wa[b][:], wav[:, :, j * 512:(j + 1) * 512], writes=[("wa", b)])
                    for nn in range(4):
                        jj = j * 4 + nn
                        for k in range(8):
                            P.mm(ps[0][:, jj * 2:jj * 2 + 2], wa[b][:, k, nn * 128:(nn + 1) * 128], scs[:, k, :],
                                 k == 0, k == 7, [("wa", b), "scs"], [("ps", 0)])
                psv = ps[0][:, 0:96].rearrange("p (j t) -> p j t", t=2)
                for t in range(2):
                    P.tt("dve", mc[:, t, :], psv[:, :, t], bcol[:], ALU.add, [("ps", 0), "bcol"], ["cols"])
                for t in range(2):
                    P.stt("dve", a1[:, t, :], mc[:, t, 8:16], 1.0, ncol[:, 0, :], ALU.add, ALU.mult, ["cols", "ncol"], ["cols"])
                    P.stt("dve", a2[:, t, :], mc[:, t, 32:40], 1.0, ncol[:, 1, :], ALU.add, ALU.mult, ["cols", "ncol"], ["cols"])
                P.emit()
            if stop == ("A", l):
                break

            with ExitStack() as ph:
                wqk = sbt(ph, "wqk", [128, 8, NQK * 128], BF16)
                wsw = sbt(ph, "wsw", [128, 8, NSW * 128], BF16)
                wv = sbt(ph, "wv", [128, 8, 768], BF16)
                qkg = sbt(ph, "qkg", [128, 4], F32)
                xc = [sbt(ph, "xc0", [128, 8, 512], F32)] * 2
                sq = sbt(ph, "sq", [128, 8, 512], F32)
                rstd = sbt(ph, "rstd", [128, 512], F32)
                hT = [sbt(ph, "hT%d" % i, [128, 8, 512], BF16) for i in range(2)]
                rp = [sbt(ph, "rp0", [128, 4, 512], F32)] * 2
                qk = [sbt(ph, "qk%d" % i, [128, 512], BF16) for i in range(4)]
                vs = [sbt(ph, "vs%d" % i, [128, 4, VWP], BF16) for i in range(2)]
                t1 = [sbt(ph, "t1_%d" % i, [128, 512], F32) for i in range(2)]
                t2 = [sbt(ph, "t2_%d" % i, [128, 512], F32) for i in range(2)]
                sqq = sbt(ph, "sqq", [128, 512], F32)
                rq = sbt(ph, "rq", [128, 512], F32)
                P.dma("pool", wqk[:], dr["wqk"][l], writes=["wqk"])
                P.dma("pool", wsw[:], dr["wsw"][l], writes=["wsw"])
                P.dma("pool", wv[:], dr["wv"][l], writes=["wv"])
                P.dma("sp", qkg[:], dr["qkg"][l], writes=["qkg"])
                for b in range(2):
                    vv = vs[b][:, :, 0:VW].rearrange("p t (h c) -> p t h c", c=65)
                    P.memset("pool", vs[b][:, :, VW:VWP], 0.0, [("vs", b)])
                    P.memset("pool", vv[:, :, :, 64:65], 1.0, [("vs", b)])
                ropev = dr["rope"].rearrange("f p t -> p f t")
                for ci, (t0, N) in enumerate(CHUNKS):
                    b = ci % 2
                    tty = 1 if ci == 0 else 0
                    P.dma("sp", xc[b][:, :, :N], XTv[:, :, t0:t0 + N], reads=xtk(ci), writes=[("xc", 0)])
                    P.dma("sp", rp[b][:, :, :N], ropev[:, :, t0:t0 + N], writes=[("rp", 0)])
                    norm_mod(xc[b], N, lambda k: a1[:, tty, k:k + 1], lambda k: mc[:, tty, k:k + 1],
                             [(lambda k: hT[b][:, k, :N], ("hT", b))], sq, rstd, 7, "B", lambda k: [("xc", 0)])
                    hkeys = [(("hT", b), k) for k in range(8)]
                    for j in range(NQK):
                        pa = j % 2
                        pq = ps[pa]
                        for k in range(8):
                            P.mm(pq[:, :N], wqk[:, k, j * 128:(j + 1) * 128], hT[b][:, k, :N], k == 0, k == 7,
                                 ["wqk", hkeys[k]], [("ps", pa)])
                        qi = (ci * NQK + j) % 4
                        dst = qk[qi][:, :N]
                        dkey = [("qk", qi)]

                        def qstore(qi=qi, j=j, t0=t0, N=N, ci=ci):
                            P.dma("pool", QKT[j * 128:(j + 1) * 128, t0:t0 + N], qk[qi][:, :N], reads=[("qk", qi)], writes=[("QKT", ci, j)])
                        if j >= NSW:
                            P.act(dst, pq[:, :N], AF.Copy, [("ps", pa)], dkey)
                            qstore()
                            continue
                        pw = ps[2 + pa]
                        for k in range(8):
                            P.mm(pw[:, :N], wsw[:, k, j * 128:(j + 1) * 128], hT[b][:, k, :N], k == 0, k == 7,
                                 ["wsw", hkeys[k]], [("ps", 2 + pa)])
                        a, bb = t1[pa], t2[pa]
                        if j < 8:
                            P.tt("dve", a[:, :N], pq[:, :N], rp[b][:, 0, :N], ALU.mult, [("ps", pa), ("rp", 0)], [("t1", pa)])
                            P.tt("dve", bb[:, :N], pw[:, :N], rp[b][:, 1, :N], ALU.mult, [("ps", 2 + pa), ("rp", 0)], [("t2", pa)])
                            P.tt("dve", dst, a[:, :N], bb[:, :N], ALU.add, [("t1", pa), ("t2", pa)], dkey)
                            qstore()
                        elif j < 12:
                            gc = 0 if j < 10 else 2
                            P.act(sqq[:, :N], pq[:, :N], AF.Square, [("ps", pa)], ["sqq"])
                            P.mm(ps[6][:, :N], blk[:], sqq[:, :N], True, True, ["blk", "sqq"], [("ps", 6)])
                            P.ts("dve", rq[:, :N], ps[6][:, :N], 1.0 / 64, EPS, ALU.mult, ALU.add, [("ps", 6)], ["rq"])
                            P.act(rq[:, :N], rq[:, :N], AF.Sqrt, ["rq"], ["rq"])
                            P.recip(rq[:, :N], rq[:, :N], ["rq"], ["rq"])
                            P.stt("dve", a[:, :N], pq[:, :N], qkg[:, gc:gc + 1], rp[b][:, 2, :N], ALU.mult, ALU.mult,
                                  [("ps", pa), ("rp", 0), "qkg"], [("t1", pa)])
                            P.stt("dve", bb[:, :N], pw[:, :N], qkg[:, gc + 1:gc + 2], rp[b][:, 3, :N], ALU.mult, ALU.mult,
                                  [("ps", 2 + pa), ("rp", 0), "qkg"], [("t2", pa)])
                            P.tt("dve", a[:, :N], a[:, :N], bb[:, :N], ALU.add, [("t1", pa), ("t2", pa)], [("t1", pa)])
                            P.tt("dve", dst, a[:, :N], rq[:, :N], ALU.mult, [("t1", pa), "rq"], dkey)
                            qstore()
                        else:
                            P.tt("dve", a[:, :N], pq[:, :N], rp[b][:, 2, :N], ALU.mult, [("ps", pa), ("rp", 0)], [("t1", pa)])
                            P.tt("dve", bb[:, :N], pw[:, :N], rp[b][:, 3, :N], ALU.mult, [("ps", 2 + pa), ("rp", 0)], [("t2", pa)])
                            P.tt("dve", dst, a[:, :N], bb[:, :N], ALU.add, [("t1", pa), ("t2", pa)], dkey)
                            qstore()
                    ntt = N // 128
                    for tt in range(ntt):
                        vv = vs[b][:, tt, 0:VW].rearrange("p (h c) -> p h c", c=65)
                        for g, (c0, cw, pb) in enumerate(((0, 512, 4), (512, 256, 5))):
                            for k in range(8):
                                P.mm(ps[pb][:, :cw], hT[b][:, k, tt * 128:(tt + 1) * 128], wv[:, k, c0:c0 + cw], k == 0, k == 7,
                                     ["wv", hkeys[k]], [("ps", pb)])
                            h0 = c0 // 64
                            P.act(vv[:, h0:h0 + cw // 64, 0:64], ps[pb][:, :cw].rearrange("p (h c) -> p h c", c=64), AF.Copy,
                                  [("ps", pb)], [("vs", b)])
                    P.dma("pool", Vv[:, t0 // 128:t0 // 128 + ntt, :], vs[b][:, :ntt, :], reads=[("vs", b)], writes=[("V", ci)])
                P.emit()
            if stop == ("B", l):
                break

            with ExitStack() as ph:
                vt = sbt(ph, "vt", [128, 34, VWP], BF16)
                rows = [sbt(ph, "rows%d" % i, [128, NT], BF16) for i in range(2)]
                qz = [[sbt(ph, "qz%d_%d" % (i, bq), [128, NT], BF16) for bq in range(2)] for i in range(2)]
                for i in range(2):
                    P.memset("pool", qz[i][0][64:128, :], 0.0, [("qzz", i, 0)])
                    P.memset("pool", qz[i][1][0:64, :], 0.0, [("qzz", i, 1)])
                pT2 = [sbt(ph, "pT%d" % i, [128, 2, 512], BF16) for i in range(3)]
                rl = sbt(ph, "rl", [128, 512], F32)
                osb = [sbt(ph, "osb%d" % i, [128, 512], F32) for i in range(2)]
                on = [sbt(ph, "on%d" % i, [128, 512], F32) for i in range(2)]
                obf = [sbt(ph, "obf%d" % i, [128, 512], BF16) for i in range(2)]
                dif = sbt(ph, "dif", [128, 512], F32)
                dsq = sbt(ph, "dsq", [128, 512], F32)
                drs = sbt(ph, "drs", [128, 512], F32)
                dm = sbt(ph, "dm", [128, 6, 512], BF16)
                t4r = sbt(ph, "t4r", [128, 16, 64], F32)
                t3r = sbt(ph, "t3r", [128, 22, 64], F32)
                t4 = [sbt(ph, "t4_%d" % i, [128, 16, 64], BF16) for i in range(4)]
                t3 = [sbt(ph, "t3_%d" % i, [128, 22, 64], BF16) for i in range(4)]
                lamt = sbt(ph, "lamt", [32, 4], F32)
                lamp = sbt(ph, "lamp", [32, 2], F32)
                lamc = sbt(ph, "lamc", [128, 4], F32)
                dgc = sbt(ph, "dgc", [64, 1], F32)
                esk = sbt(ph, "esk", [128, 4], F32)
                allchunks = list(range(9))
                P.dma("sp", vt[:], Vv, reads=[("V", ci) for ci in allchunks], writes=["vt"])
                P.dma("sp", dm[:], dr["dmask"], writes=["dm"])
                P.dma("sp", lamt[:], dr["lamT"][l], writes=["lamt"])
                P.tt("dve", lamp[:, 0:1], lamt[:, 0:1], lamt[:, 1:2], ALU.mult, ["lamt"], ["lamp"])
                P.tt("dve", lamp[:, 1:2], lamt[:, 2:3], lamt[:, 3:4], ALU.mult, ["lamt", "lamp"], ["lamp"])
                P.mm(ps[5][:, 0:2], onesf[0:32, :], lamp[:, :], True, True, ["onesf", "lamp"], [("ps", 5)])
                P.act(lamc[:, 0:2], ps[5][:, 0:2], AF.Exp, [("ps", 5)], ["lamc"])
                P.tt("dve", lamc[:, 2:3], lamc[:, 1:2], lamc[:, 0:1], ALU.subtract, ["lamc"], ["lamc"])
                P.ts("dve", lamc[:, 2:3], lamc[:, 2:3], -lam_init, None, ALU.add, None, ["lamc"], ["lamc"])
                P.dma("sp", dgc[:], dr["dgcol"][l], writes=["dgc"])
                P.ts("dve", dgc[:], dgc[:], 1.0 - lam_init, None, ALU.mult, None, ["dgc"], ["dgc"])
                P.dma("sp", esk[:], dr["sinkb"][l], writes=["esk"])
                P.act(esk[:], esk[:], AF.Exp, ["esk"], ["esk"])
                for h in range(4):
                    P.dma("sp", t4r[:], dr["nat4"][l, h], writes=["t4r"])
                    P.act(t4[h][:], t4r[:], AF.Exp, ["t4r"], [("t4", h)])
                    P.dma("sp", t3r[:], dr["nat3"][l, h], writes=["t3r"])
                    P.act(t3[h][:], t3r[:], AF.Exp, ["t3r"], [("t3", h)])

                state = {"pass": 0, "head": 0}

                def head_rows(qj, kj, bases=(0, 64)):
                    hi = state["head"] % 2
                    state["head"] += 1
                    for bq in bases:
                        P.dma("sp", qz[hi][bq // 64][bq:bq + 64, :], QKT[qj * 128 + bq:qj * 128 + bq + 64, :],
                              reads=[("QKT", ci, qj) for ci in allchunks], writes=[("qz", hi, bq // 64)])
                    P.dma("sp", rows[hi][:], QKTv[:, kj, :], reads=[("QKT", ci, kj) for ci in allchunks], writes=[("rows", hi)])
                    return hi, hi

                def attn(qslot, kslot, base, dqk, vh, scale, ci, ktiles, fin):
                    t0, N = CHUNKS[ci]
                    pi = state["pass"] % 2
                    state["pass"] += 1
                    po = ps[2 + pi]
                    nk = len(ktiles)
                    groups = [ktiles[i:i + 2] for i in range(0, nk, 2)]
                    ng = len(groups)

                    def qk(g):
                        pb0 = (0, 6)[g % 2]
                        for j, (kt, _) in enumerate(groups[g]):
                            P.mm(ps[pb0 + j][:, :N], rows[kslot][:, kt * 128:(kt + 1) * 128],
                                 qz[qslot][base // 64][:, t0:t0 + N], True, True,
                                 [("rows", kslot), ("qz", qslot, base // 64), ("qzz", qslot, base // 64)], [("ps", pb0 + j)])

                    qk(0)
                    cnt = 0
                    for g, grp in enumerate(groups):
                        pb0 = (0, 6)[g % 2]
                        pt2 = pT2[g % 3]
                        ptk = ("pT", g % 3)
                        w = len(grp)
                        if g + 1 < ng:
                            qk(g + 1)
                        src = psbig[:, pb0 * 512:(pb0 + 2) * 512].rearrange("p (b n) -> p b n", b=2)[:, :w, :N]
                        P.act(pt2[:, :w, :N], src, AF.Exp, [("ps", pb0 + j) for j in range(w)], [ptk], scale=scale)
                        for j, (kt, mask) in enumerate(grp):
                            pt = pt2[:, j, :]
                            if mask is not None:
                                mask(pt, ptk)
                            P.mm(po[:, :N], vt[:, kt, vh * 65:vh * 65 + 128], pt[:, :N], cnt == 0, cnt == nk - 1,
                                 ["vt", ptk], [("ps", 2 + pi)])
                            cnt += 1
                        if g == min(1, ng - 1) and state.get("pending") is not None:
                            pend = state["pending"]
                            state["pending"] = None
                            pend()
                    assert state.get("pending") is None
                    state["pending"] = lambda: fin(po, ("ps", 2 + pi), pi, t0, N)
                    if not ATT_DEFER:
                        state["pending"]()
                        state["pending"] = None

                def normalize(po, pokey, pi, N, dst, dkey, sink_col=None):
                    if sink_col is not None:
                        P.ts("dve", rl[64:65, :N], po[64:65, :N], sink_col, None, ALU.add, None, [pokey, "esk"], ["rl"])
                        P.recip(rl[64:65, :N], rl[64:65, :N], ["rl"], ["rl"])
                    else:
                        P.recip(rl[64:65, :N], po[64:65, :N], [pokey], ["rl"])
                    P.mm(ps[4][0:64, :N], onesf[64:65, 0:64], rl[64:65, :N], True, True, ["onesf", "rl"], [("ps", 4)])
                    P.act(osb[pi][0:64, :N], po[0:64, :N], AF.Copy, [pokey], [("osb", pi)])
                    P.tt("dve", dst, osb[pi][0:64, :N], ps[4][0:64, :N], ALU.mult, [("osb", pi), ("ps", 4)], dkey)

                def store(pi, row0, t0, N):
                    P.dma("pool", OT[row0:row0 + 64, t0:t0 + N], obf[pi][0:64, :N], reads=[("obf", pi)], writes=[("OT", row0, t0)])

                qchunks = allchunks if need_ctx else allchunks[1:]
                allk = [(kt, None) for kt in range(34)]
                ctxk = [(0, None), (1, None)]

                for h in range(4):
                    qs, ks = head_rows(h, 4 + h)
                    for ci in qchunks:
                        kts = ctxk if ci == 0 else allk
                        for m in range(2):
                            u = 2 * h + m

                            def fin(po, pokey, pi, t0, N, m=m, h=h):
                                normalize(po, pokey, pi, N, on[m][0:64, :N], [("on", m)])
                                if m == 1:
                                    P.stt("dve", dif[0:64, :N], on[1][0:64, :N], lamc[0:64, 2:3], on[0][0:64, :N], ALU.mult, ALU.add,
                                          [("on", 0), ("on", 1), "lamc"], ["dif"])
                                    P.act(dsq[0:64, :N], dif[0:64, :N], AF.Square, ["dif"], ["dsq"])
                                    P.mm(ps[5][0:64, :N], blk[0:64, 0:64], dsq[0:64, :N], True, True, ["blk", "dsq"], [("ps", 5)])
                                    P.ts("dve", drs[0:64, :N], ps[5][0:64, :N], 1.0 / 64, EPS, ALU.mult, ALU.add, [("ps", 5)], ["drs"])
                                    P.act(drs[0:64, :N], drs[0:64, :N], AF.Sqrt, ["drs"], ["drs"])
                                    P.recip(drs[0:64, :N], drs[0:64, :N], ["drs"], ["drs"])
                                    P.stt("dve", obf[pi][0:64, :N], dif[0:64, :N], dgc[:, 0:1], drs[0:64, :N], ALU.mult, ALU.mult,
                                          ["dif", "drs", "dgc"], [("obf", pi)])
                                    store(pi, h * 64, t0, N)

                            attn(qs, ks, 64 * m, 64, h, 32 ** -0.5, ci, kts, fin)

                for h in range(4):
                    qs, ks = head_rows(8 + h // 2, 10 + h // 2, (64 * (h % 2),))
                    for ci in qchunks:
                        kts = ctxk if ci == 0 else allk

                        def fin(po, pokey, pi, t0, N, h=h):
                            normalize(po, pokey, pi, N, obf[pi][0:64, :N], [("obf", pi)])
                            store(pi, 256 + h * 64, t0, N)

                        attn(qs, ks, 64 * (h % 2), 64, 4 + h // 2, 0.125, ci, kts, fin)

                for h in range(4):
                    qs, ks = head_rows(16 + h // 2, 18 + h // 2, (64 * (h % 2),))
                    for ci in qchunks:
                        if ci == 0:
                            kts = ctxk
                        else:
                            Rc = ci - 1
                            if Rc == 0:
                                tl = list(range(0, 6))
                            elif Rc == 7:
                                tl = list(range(26, 32))
                            else:
                                tl = list(range(4 * Rc - 2, 4 * Rc + 6))
                            kts = list(ctxk)
                            for t in tl:
                                if 1 <= Rc <= 6:
                                    n0 = 10 - 2 * (t - 4 * Rc)

                                    def mask(pt, ptk, n0=n0, h=h):
                                        P.tt("dve", pt[:, :], pt[:, :], t3[h][:, n0:n0 + 8, :].rearrange("p a b -> p (a b)"), ALU.mult,
                                             [ptk, ("t3", h)], [ptk])
                                else:
                                    def mask(pt, ptk, t=t, Rc=Rc, h=h):
                                        for jq in range(8):
                                            qr = 8 * Rc + jq
                                            r0 = min(max(qr - 4, 0), 56)
                                            for half in range(2):
                                                kr = 2 * t + half
                                                slot = kr - qr + 7 if r0 <= kr <= r0 + 7 else 15
                                                P.tt("dve", pt[64 * half:64 * half + 64, jq * 64:(jq + 1) * 64],
                                                     pt[64 * half:64 * half + 64, jq * 64:(jq + 1) * 64],
                                                     t4[h][64 * half:64 * half + 64, slot, :], ALU.mult, [ptk, ("t4", h)], [ptk])
                                kts.append((t + 2, mask))

                        def fin(po, pokey, pi, t0, N, h=h):
                            normalize(po, pokey, pi, N, obf[pi][0:64, :N], [("obf", pi)])
                            store(pi, 512 + h * 64, t0, N)

                        attn(qs, ks, 64 * (h % 2), 64, 6 + h, 0.125, ci, kts, fin)

                for h in range(4):
                    qs, ks = head_rows(12 + h // 2, 14 + h // 2, (64 * (h % 2),))
                    for ci in qchunks:
                        if ci == 0:
                            kts = ctxk
                        else:
                            Qc = ci - 1
                            kts = list(ctxk)
                            for o in range(6):
                                t = 4 * Qc - 1 + o
                                if t < 0 or t > 31:
                                    continue

                                def mask(pt, ptk, o=o):
                                    P.tt("dve", pt[:, :], pt[:, :], dm[:, o, :], ALU.mult, [ptk, "dm"], [ptk])
                                kts.append((t + 2, mask))

                        def fin(po, pokey, pi, t0, N, h=h):
                            normalize(po, pokey, pi, N, obf[pi][0:64, :N], [("obf", pi)], sink_col=esk[64:65, h:h + 1])
                            store(pi, 768 + h * 64, t0, N)

                        attn(qs, ks, 64 * (h % 2), 64, 10 + h // 2, 0.125, ci, kts, fin)
                if state["pending"] is not None:
                    state["pending"]()
                state["pending"] = None
                P.emit()
            if stop == ("C", l):
                break

            dchunks = list(range(9)) if need_ctx else list(range(1, 9))
            with ExitStack() as ph:
                wo = sbt(ph, "wo", [128, 8, D], BF16)
                rw = sbt(ph, "rw", [128, 8, E], F32)
                rb = sbt(ph, "rb", [128, E], F32)
                ot = [sbt(ph, "ot%d" % i, [128, 8, 512], BF16) for i in range(2)]
                xc = [sbt(ph, "dxc%d" % i, [128, 8, 512], F32) for i in range(2)]
                xn = [sbt(ph, "dxn%d" % i, [128, 8, 512], F32) for i in range(2)]
                sq = sbt(ph, "dsq2", [128, 8, 512], F32)
                rstd = sbt(ph, "drstd", [128, 512], F32)
                h2f = sbt(ph, "h2f", [128, 8, 512], F32)
                h2b = [sbt(ph, "h2b%d" % i, [128, 8, 512], BF16) for i in range(2)]
                lg = sbt(ph, "lg", [128, E], F32)
                m8 = sbt(ph, "m8", [128, 8], F32)
                nm = sbt(ph, "nm", [128, 1], F32)
                msk0 = sbt(ph, "msk", [128, E], F32)
                ex0 = sbt(ph, "ex", [128, E], F32)
                msk, ex = msk0[:], ex0[:]
                h2r = [sbt(ph, "h2r%d" % i, [128, D], BF16) for i in range(2)]
                ssum = sbt(ph, "ssum", [128, 1], F32)
                gts = [sbt(ph, "gts%d" % i, [32, 512], F32) for i in range(2)]
                P.dma("pool", wo[:], dr["wout"][l], writes=["wo"])
                P.dma("sp", rw[:], dr["router_w"][l], writes=["rw"])
                P.dma("sp", rb[:], dr["rb_rep"][l], writes=["rb"])
                for ci in dchunks:
                    t0, N = CHUNKS[ci]
                    b = ci % 2
                    tty = 1 if ci == 0 else 0
                    otk = [("OT", r0, t0) for r0 in range(0, D, 64)]
                    P.dma("sp", ot[b][:, :, :N], OTv[:, :, t0:t0 + N], reads=otk, writes=[("ot", b)])
                    P.dma("sp", xc[b][:, :, :N], XTv[:, :, t0:t0 + N], reads=xtk(ci), writes=[("dxc", b)])
                    for j in range(8):
                        pa = j % 2
                        for k in range(8):
                            P.mm(ps[pa][:, :N], wo[:, k, j * 128:(j + 1) * 128], ot[b][:, k, :N], k == 0, k == 7,
                                 ["wo", ("ot", b)], [("ps", pa)])
                        P.stt("dve", xn[b][:, j, :N], ps[pa][:, :N], mc[:, tty, 16 + j:17 + j], xc[b][:, j, :N], ALU.mult, ALU.add,
                              [("ps", pa), ("dxc", b), "cols"], [("dxn", b, j)])
                    P.dma("pool", XTv[:, :, t0:t0 + N], xn[b][:, :, :N], reads=[("dxn", b, j) for j in range(8)], writes=[("XT", ci)])
                    norm_mod(xn[b], N, lambda k: a2[:, tty, k:k + 1], lambda k: mc[:, tty, 24 + k:25 + k],
                             [(lambda k: h2f[:, k, :N], "h2f")] + ([] if SPARSE else [(lambda k: h2b[b][:, k, :N], ("h2b", b))]), sq, rstd, 7, "D",
                             lambda k: [("dxn", b, k)])
                    if not SPARSE:
                        P.dma("pool", H2Tv[:, :, t0:t0 + N], h2b[b][:, :, :N], reads=[(("h2b", b), k) for k in range(8)], writes=[("H2T", ci)])
                    for tt in range(N // 128):
                        gi = t0 // 128 + tt
                        if SPARSE:
                            msk = Mall[:, gi, :]
                            ex = Gall[:, gi, :]
                        pl = ps[2 + tt % 2]
                        plk = ("ps", 2 + tt % 2)
                        for k in range(8):
                            P.mm(pl[:, 0:E], h2f[:, k, tt * 128:(tt + 1) * 128], rw[:, k, :], k == 0, k == 7, [("h2f", k), "rw"], [plk])
                        P.tt("dve", lg[:], pl[:, 0:E], rb[:], ALU.add, [plk, "rb"], ["lg"])
                        P.op("dve", lambda e: e.max(out=m8[:], in_=lg[:]), ["lg"], ["m8"])
                        mk, ek = (("Mall", gi), ("Gall", gi)) if SPARSE else ("msk", "ex")
                        P.ts("dve", msk, lg[:], m8[:, 3:4], None, ALU.is_ge, None, ["lg", "m8"], [mk])
                        P.ts("dve", nm[:], m8[:, 0:1], -1.0, None, ALU.mult, None, ["m8"], ["nm"])
                        P.act(ex, lg[:], AF.Exp, ["lg", "nm"], [ek], bias=nm[:, 0:1])
                        P.tt("dve", ex, ex, msk, ALU.mult, [ek, mk], [ek])
                        P.op("dve", lambda e, ex=ex: e.reduce_sum(out=ssum[:], in_=ex, axis=AX.X), [ek], ["ssum"])
                        P.recip(ssum[:], ssum[:], ["ssum"], ["ssum"])
                        P.ts("dve", ex, ex, ssum[:, 0:1], None, ALU.mult, None, [ek, "ssum"], [ek])
                        if not SPARSE:
                            P.tr(ps[4][0:E, 0:128], ex, ident[:], [ek, "ident"], [("ps", 4)])
                            P.act(gts[b][:, tt * 128:(tt + 1) * 128], ps[4][0:E, 0:128], AF.Copy, [("ps", 4)], [("gts", b)])
                        else:
                            hb = gi % 2
                            for half in range(2):
                                pb = 5 + half
                                for kk in range(4):
                                    k = half * 4 + kk
                                    P.tr(ps[pb][:, kk * 128:(kk + 1) * 128], h2f[:, k, tt * 128:(tt + 1) * 128], ident[:],
                                         [("h2f", k), "ident"], [("ps", pb)])
                                if half == 0:
                                    P.cp("dve", h2r[hb][:, 0:512], ps[pb][:, :], [("ps", pb)], [("h2r", hb, 0)])
                                else:
                                    P.act(h2r[hb][:, 512:1024], ps[pb][:, :], AF.Copy, [("ps", pb)], [("h2r", hb, 1)])
                            P.dma("pool", H2R[gi * 128:(gi + 1) * 128, :], h2r[hb][:], reads=[("h2r", hb, 0), ("h2r", hb, 1)],
                                  writes=[("H2R", gi)])
                    if not SPARSE:
                        P.dma("pool", GT[:, t0:t0 + N], gts[b][:, :N], reads=[("gts", b)], writes=[("GT", ci)])
                P.emit()
            if stop == ("D", l):
                break

            if SPARSE:
                TL = list(range(34)) if need_ctx else list(range(2, 34))
                with ExitStack() as ph:
                    cnt = sbt(ph, "cnt", [E, 1], F32)
                    pad = sbt(ph, "pad", [E, 1], F32)
                    tmpe = sbt(ph, "tmpe", [E, 1], F32)
                    padbc = sbt(ph, "padbc", [E, 128], F32)
                    pstart = sbt(ph, "pstart", [128, E], F32)
                    pend = sbt(ph, "pend", [128, E], F32)
                    base = sbt(ph, "base", [128, E], F32)
                    bef = sbt(ph, "bef", [128, NBLK], F32)
                    bef2 = sbt(ph, "bef2", [128, NBLK], F32)
                    t32 = [sbt(ph, "t32_%d" % i, [128, E], F32) for i in range(2)]
                    dst = sbt(ph, "dst", [128, E], F32)
                    g8 = sbt(ph, "g8", [128, 8], F32)
                    hrow = [sbt(ph, "hrow%d" % i, [128, D], BF16) for i in range(2)]
                    for n_, i in enumerate(TL):
                        P.mm(ps[0][0:E, 0:1], Mall[:, i, :], onesf[:, 0:1], n_ == 0, n_ == len(TL) - 1, [("Mall", i), "onesf"], [("ps", 0)])
                    P.cp("dve", cnt[:], ps[0][0:E, 0:1], [("ps", 0)], ["cnt"])
                    P.ts("dve", pad[:], cnt[:], 0.0, float(MB), ALU.is_gt, ALU.mult, ["cnt"], ["pad"])
                    for j in range(1, NT // MB + 1):
                        P.ts("dve", tmpe[:], cnt[:], float(MB * j), float(MB), ALU.is_gt, ALU.mult, ["cnt", "tmpe"], ["tmpe"])
                        P.tt("dve", pad[:], pad[:], tmpe[:], ALU.add, ["pad", "tmpe"], ["pad"])
                    P.ts("dve", padbc[:], onesf[0:E, :], pad[:, 0:1], None, ALU.mult, None, ["onesf", "pad"], ["padbc"])
                    P.mm(ps[1][:, 0:E], padbc[:], su32[:], True, True, ["padbc", "su32"], [("ps", 1)])
                    P.mm(ps[1][:, E:2 * E], padbc[:], ident[0:E, 0:E], True, True, ["padbc", "ident"], [("ps", 1)])
                    P.cp("dve", pstart[:], ps[1][:, 0:E], [("ps", 1)], ["pstart"])
                    P.tt("dve", pend[:], pstart[:], ps[1][:, E:2 * E], ALU.add, ["pstart", ("ps", 1)], ["pend"])
                    for bk in range(NBLK):
                        tb = t32[bk % 2]
                        P.ts("dve", tb[:], pend[:], float(bk * MB), None, ALU.is_le, None, ["pend"], [("t32", bk % 2)])
                        P.op("dve", lambda e, tb=tb, bk=bk: e.reduce_sum(out=bef[:, bk:bk + 1], in_=tb[:], axis=AX.X), [("t32", bk % 2)], ["bef"])
                    P.ts("dve", bef[:], bef[:], float(E - 1), float(E * l), ALU.min, ALU.add, ["bef"], ["bef"])
                    P.cp("dve", EIDX[:], bef[:], ["bef"], ["EIDX"])
                    P.ts("dve", bef2[:], bef[:], 128.0, iotac[:, 0:1], ALU.mult, ALU.add, ["bef", "iotac"], ["bef2"])
                    P.cp("dve", BIDX[:], bef2[:], ["bef2"], ["BIDX"])
                    w8f = sbt(ph, "w8f", [128, NBLK, 8], F32)
                    for k in range(8):
                        P.ts("dve", w8f[:, :, k], bef2[:], 8.0, float(k), ALU.mult, ALU.add, ["bef2"], ["w8f"])
                    P.cp("dve", W8i[:], w8f[:], ["w8f"], ["BIDX"])
                    for k in range(4):
                        P.ts("dve", w8f[:, :, k], bef2[:], 4.0, float(k), ALU.mult, ALU.add, ["bef2", "w8f"], ["w8f"])
                    P.cp("dve", W4i[:], w8f[:, :, 0:4], ["w8f"], ["BIDX"])
                    P.cp("dve", base[:], pstart[:], ["pstart"], ["base"])
                    for i in TL:
                        P.mm(ps[2][:, 0:E], triu[:], Mall[:, i, :], True, True, ["triu", ("Mall", i)], [("ps", 2)])
                        P.mm(ps[2][:, E:2 * E], onesf[:], Mall[:, i, :], True, True, ["onesf", ("Mall", i)], [("ps", 2)])
                        P.tt("dve", dst[:], ps[2][:, 0:E], base[:], ALU.add, [("ps", 2), "base"], ["dst"])
                        P.tt("dve", base[:], base[:], ps[2][:, E:2 * E], ALU.add, [("ps", 2), "base"], ["base"])
                        P.op("dve", lambda e, i=i: e.max(out=g8[:], in_=Gall[:, i, :]), [("Gall", i)], ["g8"])
                        P.cp("dve", G4[:, i, :], g8[:, 0:4], ["g8"], [("G4", i)])
                        for k in range(4):
                            tb = t32[k % 2]
                            P.ts("dve", tb[:], Gall[:, i, :], g8[:, k:k + 1], None, ALU.is_equal, None, [("Gall", i), "g8"], [("t32", k % 2)])
                            P.tt("dve", tb[:], tb[:], dst[:], ALU.mult, [("t32", k % 2), "dst"], [("t32", k % 2)])
                            P.op("dve", lambda e, tb=tb, i=i, k=k: e.reduce_sum(out=D4f[:, i, k:k + 1], in_=tb[:], axis=AX.X),
                                 [("t32", k % 2)], [("D4f", i)])
                        P.cp("dve", D4i[:, i, :], D4f[:, i, :], [("D4f", i)], [("D4i", i)])
                        hb = i % 2
                        P.dma("sp", hrow[hb][:], H2R[i * 128:(i + 1) * 128, :], reads=[("H2R", i)], writes=[("hrow", hb)])
                        for k in range(4):
                            P.scatter(XB, hrow[hb][:], D4i[:, i, k:k + 1], [("hrow", hb), ("D4i", i)] + [("XBr", bk) for bk in range(0)],
                                      [("XB", i, k)])
                    P.emit()
                if stop == ("E1", l):
                    with ExitStack() as ph:
                        dbg = sbt(ph, "dbg", [128, 34 * 4 + 34 * 4 + 2 * NBLK], F32)
                        P.cp("dve", dbg[:, 0:136], D4f[:].rearrange("p a b -> p (a b)"), [("D4f", i) for i in TL], ["dbg"])
                        P.cp("dve", dbg[:, 136:272], G4[:].rearrange("p a b -> p (a b)"), [("G4", i) for i in TL], ["dbg"])
                        P.cp("dve", dbg[:, 272:272 + NBLK], BIDX[:], ["BIDX"], ["dbg"])
                        P.cp("dve", dbg[:, 272 + NBLK:272 + 2 * NBLK], EIDX[:], ["EIDX"], ["dbg"])
                        P.dma("sp", DBG, dbg[:], reads=["dbg"], writes=[("XB", "dbg")])
                        P.emit()
                    break
                xbkeys = [("XB", i, k) for i in TL for k in range(4)]
                with ExitStack() as ph:
                    nsub = MB // 128
                    wg = [sbt(ph, "swg%d" % i, [128, 8, 2048], BF16) for i in range(2)]
                    wd = [sbt(ph, "swd%d" % i, [128, 8, D], BF16) for i in range(2)]
                    bg = [sbt(ph, "sbg%d" % i, [128, 16], F32) for i in range(2)]
                    bdb = [sbt(ph, "sbdb%d" % i, [128, D], F32) for i in range(2)]
                    xin = [sbt(ph, "sxin%d" % i, [128, nsub, D], BF16) for i in range(2)]
                    xT = [sbt(ph, "sxT%d" % i, [128, 8, MB], BF16) for i in range(2)]
                    aT = [sbt(ph, "saT%d" % i, [128, 8, MB], BF16) for i in range(2)]
                    gs = [sbt(ph, "sgs%d" % i, [128, MB], F32) for i in range(2)]
                    sg = [sbt(ph, "ssg%d" % i, [128, MB], F32) for i in range(2)]
                    us = [sbt(ph, "sus%d" % i, [128, MB], F32) for i in range(2)]
                    ysb = [sbt(ph, "sysb%d" % i, [128, D], F32) for i in range(2)]
                    idb = sbt(ph, "sidb", [128, 128], BF16)
                    P.cp("dve", idb[:], ident[:], ["ident"], ["idb"])
                    wguv = dr["wgu"].rearrange("l e p k n -> (l e p k) n")
                    wdnv = dr["wdn"].rearrange("l e p (j a) n -> (l e p j) (a n)", a=2)
                    bguv = dr["bgu_col"].rearrange("l e p f -> (l e p) f")
                    bdnv = dr["bdn"].rearrange("l e d -> (l e) d")
                    XBv = XB.rearrange("(b s p) d -> b p s d", s=nsub, p=128)
                    for bk in range(NBLK):
                        wb = bk % 2
                        for k in range(8):
                            P.gather(wg[wb][:, k, :], wguv, W8i[:, bk, k:k + 1], ["BIDX"], [("wg", wb)] if k == 0 else [("wgx", wb, k)])
                        for j in range(4):
                            P.gather(wd[wb][:, 2 * j:2 * j + 2, :].rearrange("p a n -> p (a n)"), wdnv, W4i[:, bk, j:j + 1], ["BIDX"],
                                     [("wd", wb)] if j == 0 else [("wdx", wb, j)])
                        P.gather(bg[wb][:], bguv, BIDX[:, bk:bk + 1], ["BIDX"], [("bg", wb)])
                        P.gather(bdb[wb][:], bdnv, EIDX[:, bk:bk + 1], ["EIDX"], [("bdb", wb)])
                        P.dma("sp", xin[wb][:], XBv[bk], reads=xbkeys, writes=[("xin", wb)])
                        for s_ in range(nsub):
                            pb = 6 + s_ % 2
                            pbf = ps[pb][:].bitcast(BF16)
                            for k in range(8):
                                P.tr(pbf[:, k * 128:(k + 1) * 128], xin[wb][:, s_, k * 128:(k + 1) * 128], idb[:], [("xin", wb), "idb"], [("ps", pb)])
                            src = pbf[:, 0:1024].rearrange("p (k j) -> p k j", k=8)
                            if s_ % 2 == 0:
                                P.cp("dve", xT[wb][:, :, s_ * 128:(s_ + 1) * 128], src, [("ps", pb)], [("xT", wb, s_)])
                            else:
                                P.act(xT[wb][:, :, s_ * 128:(s_ + 1) * 128], src, AF.Copy, [("ps", pb)], [("xT", wb, s_)])
                        xk = [("xT", wb, s_) for s_ in range(nsub)]
                        wgk = [("wg", wb)] + [("wgx", wb, k) for k in range(1, 8)]
                        wdk = [("wd", wb)] + [("wdx", wb, j) for j in range(1, 4)]
                        for fi in range(8):
                            pg, pu = ps[(2 * fi) % 4], ps[(2 * fi + 1) % 4]
                            pgk, puk = ("ps", (2 * fi) % 4), ("ps", (2 * fi + 1) % 4)
                            fb = fi % 2
                            for k in range(8):
                                P.mm(pg[:, :MB], wg[wb][:, k, fi * 128:(fi + 1) * 128], xT[wb][:, k, :], k == 0, k == 7, wgk + xk, [pgk])
                            for k in range(8):
                                P.mm(pu[:, :MB], wg[wb][:, k, (8 + fi) * 128:(9 + fi) * 128], xT[wb][:, k, :], k == 0, k == 7, wgk + xk, [puk])
                            P.ts("dve", gs[fb][:], pg[:, :MB], bg[wb][:, fi:fi + 1], 7.0, ALU.add, ALU.min, [pgk, ("bg", wb)], [("gs", fb)])
                            P.act(sg[fb][:], gs[fb][:], AF.Sigmoid, [("gs", fb)], [("sg", fb)], scale=1.702)
                            P.act(us[fb][:], pu[:, :MB], AF.Identity, [puk, ("bg", wb)], [("us", fb)], bias=bg[wb][:, 8 + fi:9 + fi])
                            P.ts("dve", us[fb][:], us[fb][:], 7.0, -7.0, ALU.min, ALU.max, [("us", fb)], [("us", fb)])
                            P.tt("dve", gs[fb][:], gs[fb][:], sg[fb][:], ALU.mult, [("gs", fb), ("sg", fb)], [("gs", fb)])
                            P.stt("dve", aT[wb][:, fi, :], us[fb][:], 1.0, gs[fb][:], ALU.add, ALU.mult, [("gs", fb), ("us", fb)], [("aT", wb, fi)])
                        for s_ in range(nsub):
                            yb = (bk * nsub + s_) % 2
                            for half in range(2):
                                pd = ps[4 + half]
                                pdk = ("ps", 4 + half)
                                for f in range(8):
                                    P.mm(pd[:, :], aT[wb][:, f, s_ * 128:(s_ + 1) * 128], wd[wb][:, f, half * 512:(half + 1) * 512], f == 0, f == 7,
                                         wdk + [("aT", wb, f)], [pdk])
                                P.tt("dve", ysb[yb][:, half * 512:(half + 1) * 512], pd[:, :], bdb[wb][:, half * 512:(half + 1) * 512], ALU.add,
                                     [pdk, ("bdb", wb)], [("ysb", yb, half)])
                            r0 = bk * MB + s_ * 128
                            P.dma("sp", YB[r0:r0 + 128, :], ysb[yb][:], reads=[("ysb", yb, 0), ("ysb", yb, 1)], writes=[("YB", bk, s_)])
                    P.emit()
                if stop == ("E2", l):
                    break
                ybkeys = [("YB", bk, s_) for bk in range(NBLK) for s_ in range(MB // 128)]
                with ExitStack() as ph:
                    yk = [[sbt(ph, "yk%d_%d" % (i, k), [128, D], F32) for k in range(4)] for i in range(2)]
                    yacc = [sbt(ph, "yacc%d" % i, [128, D], F32) for i in range(2)]
                    xe3 = [sbt(ph, "xe3_%d" % i, [128, 8, 512], F32) for i in range(2)]
                    yT = sbt(ph, "yT", [128, 8, 512], F32)
                    for ci in dchunks:
                        t0, N = CHUNKS[ci]
                        cb = ci % 2
                        tty = 1 if ci == 0 else 0
                        P.dma("sp", xe3[cb][:, :, :N], XTv[:, :, t0:t0 + N], reads=xtk(ci), writes=[("xe3", cb, k) for k in range(8)])
                        for tt in range(N // 128):
                            gi = t0 // 128 + tt
                            yb = gi % 2
                            for k in range(4):
                                P.gather(yk[yb][k][:], YB, D4i[:, gi, k:k + 1], ybkeys + [("D4i", gi)], [("yk", yb, k)])
                            P.ts("dve", yacc[yb][:], yk[yb][0][:], G4[:, gi, 0:1], None, ALU.mult, None, [("yk", yb, 0), ("G4", gi)], [("yacc", yb)])
                            for k in range(1, 4):
                                P.stt("dve", yacc[yb][:], yk[yb][k][:], G4[:, gi, k:k + 1], yacc[yb][:], ALU.mult, ALU.add,
                                      [("yk", yb, k), ("G4", gi), ("yacc", yb)], [("yacc", yb)])
                            for half in range(2):
                                pb = (2 * tt + half) % 4
                                for kk in range(4):
                                    k = half * 4 + kk
                                    P.tr(ps[pb][:, kk * 128:(kk + 1) * 128], yacc[yb][:, k * 128:(k + 1) * 128], ident[:],
                                         [("yacc", yb), "ident"], [("ps", pb)])
                                dstv = yT[:, half * 4:(half + 1) * 4, tt * 128:(tt + 1) * 128]
                                srcv = ps[pb][:, 0:512].rearrange("p (a b) -> p a b", a=4)
                                if half == 0:
                                    P.cp("dve", dstv, srcv, [("ps", pb)], [("yT", tt, half)])
                                else:
                                    P.act(dstv, srcv, AF.Copy, [("ps", pb)], [("yT", tt, half)])
                        ytk = [("yT", tt, half) for tt in range(N // 128) for half in range(2)]
                        for k in range(8):
                            P.stt("dve", xe3[cb][:, k, :N], yT[:, k, :N], mc[:, tty, 40 + k:41 + k], xe3[cb][:, k, :N], ALU.mult, ALU.add,
                                  ytk + [("xe3", cb, k), "cols"], [("xe3", cb, k)] + [("yTr", k)])
                        P.dma("pool", XTv[:, :, t0:t0 + N], xe3[cb][:, :, :N], reads=[("xe3", cb, k) for k in range(8)], writes=[("XT", ci)])
                    P.emit()
                if stop == ("E", l):
                    break
                continue

            groups = [[0, 1], [2, 3], [4, 5], [6, 7], [8]] if need_ctx else [[1, 2], [3, 4], [5, 6], [7, 8]]
            with ExitStack() as ph:
                wg = [sbt(ph, "wg%d" % i, [128, 8, 2048], BF16) for i in range(2)]
                wd = [sbt(ph, "wd%d" % i, [128, 8, D], BF16) for i in range(2)]
                bg = [sbt(ph, "bg%d" % i, [128, 16], F32) for i in range(2)]
                bd = [sbt(ph, "bd%d" % i, [128, 8], F32) for i in range(2)]
                h2 = sbt(ph, "h2", [128, 8, 1024], BF16)
                acc = sbt(ph, "acc", [128, 8, 1024], F32)
                aT = [sbt(ph, "aT%d" % i, [128, 8, 512], BF16) for i in range(2)]
                gb = [sbt(ph, "gb%d" % i, [128, 512], F32) for i in range(2)]
                gs = [sbt(ph, "gs%d" % i, [128, 512], F32) for i in range(2)]
                sg = [sbt(ph, "sg%d" % i, [128, 512], F32) for i in range(2)]
                us = [sbt(ph, "us%d" % i, [128, 512], F32) for i in range(2)]
                yt = [sbt(ph, "yt%d" % i, [128, 512], F32) for i in range(2)]
                xe = [sbt(ph, "xe%d" % i, [128, 512], F32) for i in range(2)]
                it = 0
                itc = 0
                for gi, grp in enumerate([] if SPARSE else groups):
                    offs = {}
                    o = 0
                    for ci in grp:
                        t0, N = CHUNKS[ci]
                        offs[ci] = o
                        P.dma("sp", h2[:, :, o:o + N], H2Tv[:, :, t0:t0 + N], reads=[("H2T", ci)], writes=[("h2", ci)])
                        o += N
                    pend_down = None
                    for e_ in range(E):
                        wb = it % 2
                        it += 1
                        wgv = dr["wgu"][l, e_]
                        wdv = dr["wdn"][l, e_]
                        P.dma("pool", wg[wb][:], wgv, writes=[("wg", wb)])
                        P.dma("pool", wd[wb][:], wdv, writes=[("wd", wb)])
                        P.dma("sp", bg[wb][:], dr["bgu_col"][l, e_], writes=[("bg", wb)])
                        P.dma("sp", bd[wb][:], dr["bdn_col"][l, e_], writes=[("bd", wb)])
                        for ci in grp:
                            t0, N = CHUNKS[ci]
                            o = offs[ci]
                            cb = itc % 2
                            itc += 1
                            P.dma("sp", gb[cb][:, :N], GT[e_:e_ + 1, t0:t0 + N].partition_broadcast(128), reads=[("GT", ci)],
                                  writes=[("gb", cb)])
                            for fi in range(8):
                                pg, pu = ps[(2 * fi) % 4], ps[(2 * fi + 1) % 4]
                                pgk, puk = ("ps", (2 * fi) % 4), ("ps", (2 * fi + 1) % 4)
                                fb = fi % 2
                                for k in range(8):
                                    P.mm(pg[:, :N], wg[wb][:, k, fi * 128:(fi + 1) * 128], h2[:, k, o:o + N], k == 0, k == 7,
                                         [("wg", wb), ("h2", ci)], [pgk])
                                for k in range(8):
                                    P.mm(pu[:, :N], wg[wb][:, k, (8 + fi) * 128:(9 + fi) * 128], h2[:, k, o:o + N], k == 0, k == 7,
                                         [("wg", wb), ("h2", ci)], [puk])
                                P.ts("dve", gs[fb][:, :N], pg[:, :N], bg[wb][:, fi:fi + 1], 7.0, ALU.add, ALU.min, [pgk, ("bg", wb)], [("gs", fb)])
                                P.act(sg[fb][:, :N], gs[fb][:, :N], AF.Sigmoid, [("gs", fb)], [("sg", fb)], scale=1.702)
                                P.act(us[fb][:, :N], pu[:, :N], AF.Identity, [puk, ("bg", wb)], [("us", fb)], bias=bg[wb][:, 8 + fi:9 + fi])
                                P.ts("dve", us[fb][:, :N], us[fb][:, :N], 7.0, -7.0, ALU.min, ALU.max, [("us", fb)], [("us", fb)])
                                P.tt("dve", gs[fb][:, :N], gs[fb][:, :N], sg[fb][:, :N], ALU.mult, [("gs", fb), ("sg", fb)], [("gs", fb)])
                                P.stt("dve", aT[cb][:, fi, :N], us[fb][:, :N], 1.0, gs[fb][:, :N], ALU.add, ALU.mult,
                                      [("gs", fb), ("us", fb)], [("aT", cb, fi)])
                                if fi == 1 and pend_down is not None:
                                    pd_ = pend_down
                                    pend_down = None
                                    pd_()

                            def down(e_=e_, wb=wb, ci=ci, N=N, o=o, cb=cb):
                                for dj in range(8):
                                    pd = ps[4 + dj % 2]
                                    pdk = ("ps", 4 + dj % 2)
                                    yb = dj % 2
                                    for f in range(8):
                                        P.mm(pd[:, :N], wd[wb][:, f, dj * 128:(dj + 1) * 128], aT[cb][:, f, :N], f == 0, f == 7,
                                             [("wd", wb), ("aT", cb, f)], [pdk])
                                    if e_ == 0:
                                        P.stt("dve", acc[:, dj, o:o + N], pd[:, :N], bd[wb][:, dj:dj + 1], gb[cb][:, :N], ALU.add, ALU.mult,
                                              [pdk, ("bd", wb), ("gb", cb)], [("acc", ci, dj)])
                                    else:
                                        P.stt("dve", yt[yb][:, :N], pd[:, :N], bd[wb][:, dj:dj + 1], gb[cb][:, :N], ALU.add, ALU.mult,
                                              [pdk, ("bd", wb), ("gb", cb)], [("yt", yb)])
                                        P.tt("dve", acc[:, dj, o:o + N], acc[:, dj, o:o + N], yt[yb][:, :N], ALU.add,
                                             [("yt", yb), ("acc", ci, dj)], [("acc", ci, dj)])
                            pend_down = down
                            if not MOE_DEFER:
                                pend_down()
                                pend_down = None
                    if pend_down is not None:
                        pend_down()
                    for ci in grp:
                        t0, N = CHUNKS[ci]
                        o = offs[ci]
                        tty = 1 if ci == 0 else 0
                        for dj in range(8):
                            xb = dj % 2
                            P.dma("sp", xe[xb][:, :N], XT[dj * 128:(dj + 1) * 128, t0:t0 + N], reads=[("XT", ci), ("XTe", ci, dj)], writes=[("xe", xb)])
                            P.stt("dve", xe[xb][:, :N], acc[:, dj, o:o + N], mc[:, tty, 40 + dj:41 + dj], xe[xb][:, :N], ALU.mult, ALU.add,
                                  [("acc", ci, dj), ("xe", xb), "cols"], [("xe", xb)])
                            P.dma("pool", XT[dj * 128:(dj + 1) * 128, t0:t0 + N], xe[xb][:, :N], reads=[("xe", xb), ("XT", ci)], writes=[("XTe", ci, dj)])
                P.emit()
            if stop == ("E", l):
                break

        if stop is None:
            with ExitStack() as ph:
                fg = sbt(ph, "fg", [128, 8], F32)
                xc = [sbt(ph, "fxc%d" % i, [128, 8, 512], F32) for i in range(2)]
                sq = sbt(ph, "fsq", [128, 8, 512], F32)
                yf = sbt(ph, "fy", [128, 8, 512], F32)
                rstd = sbt(ph, "frstd", [128, 512], F32)
                ob = [sbt(ph, "fob%d" % i, [128, 4, D], F32) for i in range(2)]
                P.dma("sp", fg[:], dr["fgcol"], writes=["cols"])
                for ci in range(1, 9):
                    t0, N = CHUNKS[ci]
                    b = ci % 2
                    P.dma("sp", xc[b][:, :, :N], XTv[:, :, t0:t0 + N], reads=xtk(ci), writes=[("fxc", b)])
                    norm_mod(xc[b], N, lambda k: fg[:, k:k + 1], None, [(lambda k: yf[:, k, :N], "fy")], sq, rstd, 7, "F",
                             lambda k: [("fxc", b)])
                    for tt in range(4):
                        for half in range(2):
                            pb = (2 * tt + half) % 4
                            for kk in range(4):
                                k = half * 4 + kk
                                P.tr(ps[pb][:, kk * 128:(kk + 1) * 128], yf[:, k, tt * 128:(tt + 1) * 128], ident[:],
                                     [("fy", k), "ident"], [("ps", pb)])
                            if half == 0:
                                P.cp("dve", ob[b][:, tt, 0:512], ps[pb][:, :], [("ps", pb)], [("fob", b, tt, 0)])
                            else:
                                P.act(ob[b][:, tt, 512:1024], ps[pb][:, :], AF.Copy, [("ps", pb)], [("fob", b, tt, 1)])
                    P.dma("sp", out[t0 - C:t0 - C + N, :].rearrange("(t p) d -> p t d", p=128), ob[b][:],
                          reads=[("fob", b, tt, hh) for tt in range(4) for hh in range(2)], writes=[("out", ci)])
                P.wait_all("sp", [("out", ci) for ci in range(1, 9)])
                P.emit()
        else:
            keys = [k for k in P.state.keys() if isinstance(k, tuple) and k[0] in ("XT", "XTe", "QKT", "V", "OT", "H2T", "GT", "H2R", "XB", "YB")]
            P.wait_all("sp", keys)
            P.wait_all("pool", keys)
            P.emit()
    return nc


_DT = {np.dtype(np.float32): F32, np.dtype(ml_dtypes.bfloat16): BF16}


def _run(inputs, n_layers=L, debug=False, stop=None, cores=8):
    inp = {k: np.asarray(v) for k, v in inputs.items()}
    sh = _prep_shared(inp)
    in_maps = []
    for b in range(cores):
        m = dict(sh)
        m["x"] = np.ascontiguousarray(inp["x"][b])
        m["ctx"] = np.ascontiguousarray(inp["ctx"][b])
        cv = np.stack([inp["c"][b].reshape(8, 128).T, inp["c_ctx"].reshape(8, 128).T], axis=-1)
        m["cvec"] = np.ascontiguousarray(cv.astype(np.float32))
        in_maps.append(m)
    shapes = {k: (v.shape, _DT[v.dtype]) for k, v in in_maps[0].items()}
    nc = build_program(shapes, n_layers=n_layers, debug=debug, stop=stop)
    res = run_bass_kernel_spmd(nc, in_maps, core_ids=list(range(cores)))
    return res


def kernel(**inputs):
    res = _run(inputs)
    return np.stack([np.asarray(r["out"], dtype=np.float32) for r in res.results], axis=0)
```
